# Optimizing a Trainium2 kernel written in Bass

```python
import math
import jax
import jax.numpy as jnp
from jax import lax
import numpy as np

D_MODEL = 1024
BATCH = 32
SEQ = 2048
DEPTH = 1

SSM_EXPAND = 2
SSM_D_INNER = SSM_EXPAND * D_MODEL
SSM_HEAD_DIM = 64
SSM_HEADS = SSM_D_INNER // SSM_HEAD_DIM
SSM_GROUPS = 4
SSM_HPG = SSM_HEADS // SSM_GROUPS
SSM_STATE = 128
SSM_CONV = 4
SSM_CHUNK = 128
SSM_CONV_DIM = SSM_D_INNER + 2 * SSM_GROUPS * SSM_STATE

NSA_HEADS = 16
NSA_HEAD_DIM = 64
NSA_WIDTH = NSA_HEADS * NSA_HEAD_DIM
NSA_KV_GROUPS = 2
NSA_HPG = NSA_HEADS // NSA_KV_GROUPS
NSA_KV_WIDTH = NSA_KV_GROUPS * NSA_HEAD_DIM
CMP_BLOCK = 32
CMP_STRIDE = 16
CMP_HIDDEN = 256
SEL_BLOCK = 64
SEL_TOPK = 4
WINDOW = 512
NSA_QBLOCK = 64
ROPE_THETA = 10000.0

MOE_GROUPS = 4
EXPERTS_PER_GROUP = 8
N_EXPERTS = MOE_GROUPS * EXPERTS_PER_GROUP
EXPERT_TOPK = 2
EXPERT_HIDDEN = 256

NORM_EPS = 1e-6

IN_SIZES = (SSM_D_INNER, SSM_CONV_DIM, SSM_HEADS, NSA_WIDTH, NSA_KV_WIDTH, NSA_KV_WIDTH, NSA_KV_WIDTH, NSA_KV_WIDTH, NSA_KV_WIDTH, NSA_KV_WIDTH, 3 * NSA_HEADS, D_MODEL, D_MODEL)
IN_WIDTH = SSM_D_INNER + SSM_CONV_DIM + SSM_HEADS + NSA_WIDTH + 6 * NSA_KV_WIDTH + 3 * NSA_HEADS + 2 * D_MODEL

kernel_name = 'hybrid_ssd_nsa_hmoe_adaln_block'


def _rmsnorm(x, g):
    xf = x.astype(jnp.float32)
    y = xf * lax.rsqrt(jnp.mean(xf * xf, axis=-1, keepdims=True) + NORM_EPS)
    return (y * g.astype(jnp.float32)).astype(x.dtype)


def _masked_softmax(s, mask):
    s = jnp.where(mask, s.astype(jnp.float32), -1e30)
    m = jnp.max(s, axis=-1, keepdims=True)
    p = jnp.where(mask, jnp.exp(s - m), 0.0)
    return p / jnp.maximum(jnp.sum(p, axis=-1, keepdims=True), 1e-30)


def _rope(t, cos, sin):
    shape = cos.shape[:2] + (1,) * (t.ndim - 3) + cos.shape[2:]
    cos = cos.reshape(shape)
    sin = sin.reshape(shape)
    t1, t2 = jnp.split(t.astype(jnp.float32), 2, axis=-1)
    return jnp.concatenate([t1 * cos - t2 * sin, t2 * cos + t1 * sin], axis=-1).astype(t.dtype)


def _ssd_chunked_scan(xh, dt, a, bm, cm):
    b, s = xh.shape[:2]
    nc = s // SSM_CHUNK

    def chunks(t):
        t = t.astype(jnp.float32)
        return jnp.moveaxis(t.reshape((b, nc, SSM_CHUNK) + t.shape[2:]), 1, 0)

    tril = jnp.tril(jnp.ones((SSM_CHUNK, SSM_CHUNK), dtype=bool))[None, :, :, None, None]
    a = a.astype(jnp.float32)

    def step(state, inp):
        xc, dtc, bc, cc = inp
        acum = jnp.cumsum(dtc * a, axis=1)
        seg = acum[:, :, None] - acum[:, None, :]
        decay = jnp.exp(jnp.where(tril, seg, -jnp.inf))
        cb = jnp.einsum('btgn,bsgn->bgts', cc, bc)
        y_diag = jnp.einsum('bgts,btsgr,bsgr,bsgrp->btgrp', cb, decay, dtc, xc)
        y_off = jnp.einsum('btgn,bgrpn->btgrp', cc, state) * jnp.exp(acum)[..., None]
        w_end = jnp.exp(acum[:, -1:] - acum) * dtc
        new_state = state * jnp.exp(acum[:, -1])[..., None, None] + jnp.einsum('bsgn,bsgr,bsgrp->bgrpn', bc, w_end, xc)
        return new_state, y_diag + y_off

    state0 = jnp.zeros((b, SSM_GROUPS, SSM_HPG, SSM_HEAD_DIM, SSM_STATE), jnp.float32)
    _, ys = lax.scan(step, state0, (chunks(xh), chunks(dt), chunks(bm), chunks(cm)))
    return jnp.moveaxis(ys, 0, 1).reshape(xh.shape).astype(xh.dtype)


def _mamba2_mixer(h_z, h_xbc, h_dt, conv_w, conv_b, dt_bias, a_log, d_skip, ssm_norm_g):
    b, s, _ = h_xbc.shape
    xbc = lax.conv_general_dilated(h_xbc, conv_w[:, None, :], window_strides=(1,), padding=[(SSM_CONV - 1, 0)], dimension_numbers=('NWC', 'WIO', 'NWC'), feature_group_count=SSM_CONV_DIM)
    xbc = jax.nn.silu(xbc + conv_b)
    xs, bm, cm = jnp.split(xbc, [SSM_D_INNER, SSM_D_INNER + SSM_GROUPS * SSM_STATE], axis=-1)
    xh = xs.reshape(b, s, SSM_GROUPS, SSM_HPG, SSM_HEAD_DIM)
    bm = bm.reshape(b, s, SSM_GROUPS, SSM_STATE)
    cm = cm.reshape(b, s, SSM_GROUPS, SSM_STATE)
    dt = jax.nn.softplus((h_dt + dt_bias).astype(jnp.float32)).reshape(b, s, SSM_GROUPS, SSM_HPG)
    a = -jnp.exp(a_log.astype(jnp.float32)).reshape(SSM_GROUPS, SSM_HPG)
    y = _ssd_chunked_scan(xh, dt, a, bm, cm) + d_skip.reshape(SSM_GROUPS, SSM_HPG, 1) * xh
    y = y.reshape(b, s, SSM_D_INNER) * jax.nn.silu(h_z)
    yg = y.astype(jnp.float32).reshape(b, s, SSM_GROUPS, SSM_D_INNER // SSM_GROUPS)
    yg = yg * lax.rsqrt(jnp.mean(yg * yg, axis=-1, keepdims=True) + NORM_EPS)
    return (yg.reshape(b, s, SSM_D_INNER) * ssm_norm_g).astype(h_z.dtype)


def _compress(t, pe, w1, w2, idx):
    b = t.shape[0]
    blk = t[:, idx] + pe[None, None, :, None, :]
    blk = jnp.moveaxis(blk, 3, 2).reshape(b, idx.shape[0], NSA_KV_GROUPS, CMP_BLOCK * NSA_HEAD_DIM)
    return jax.nn.silu(blk @ w1) @ w2


def _nsa_mixer(q, kc, vc, ksl, vsl, kw, vw, gates, cos, sin, cmp_pe_k, cmp_w1_k, cmp_w2_k, cmp_pe_v, cmp_w1_v, cmp_w2_v):
    b, s = q.shape[:2]
    G, R, dk = NSA_KV_GROUPS, NSA_HPG, NSA_HEAD_DIM
    q = _rope(q.reshape(b, s, G, R, dk), cos, sin)
    kc = _rope(kc.reshape(b, s, G, dk), cos, sin)
    ksl = _rope(ksl.reshape(b, s, G, dk), cos, sin)
    kw = _rope(kw.reshape(b, s, G, dk), cos, sin)
    vc = vc.reshape(b, s, G, dk)
    vsl = vsl.reshape(b, s, G, dk)
    vw = vw.reshape(b, s, G, dk)
    gates = jax.nn.sigmoid(gates.astype(jnp.float32)).reshape(b, s, G, R, 3)
    n_cmp = (s - CMP_BLOCK) // CMP_STRIDE + 1
    cidx = jnp.arange(n_cmp)[:, None] * CMP_STRIDE + jnp.arange(CMP_BLOCK)[None, :]
    kcmp = _compress(kc, cmp_pe_k, cmp_w1_k, cmp_w2_k, cidx)
    vcmp = _compress(vc, cmp_pe_v, cmp_w1_v, cmp_w2_v, cidx)
    cstart = jnp.arange(n_cmp) * CMP_STRIDE
    cmp_end = cstart + CMP_BLOCK - 1
    n_sel = s // SEL_BLOCK
    sstart = jnp.arange(n_sel) * SEL_BLOCK
    overlap = ((cstart[:, None] < sstart[None, :] + SEL_BLOCK) & (cstart[:, None] + CMP_BLOCK > sstart[None, :])).astype(jnp.float32)
    ksb = jnp.moveaxis(ksl.reshape(b, n_sel, SEL_BLOCK, G, dk), 3, 1)
    vsb = jnp.moveaxis(vsl.reshape(b, n_sel, SEL_BLOCK, G, dk), 3, 1)
    kwp = jnp.pad(kw, ((0, 0), (WINDOW, 0), (0, 0), (0, 0)))
    vwp = jnp.pad(vw, ((0, 0), (WINDOW, 0), (0, 0), (0, 0)))
    scale = NSA_HEAD_DIM ** -0.5
    bi = jnp.arange(b)[:, None, None, None]
    gi = jnp.arange(G)[None, :, None, None]
    sel_j = jnp.arange(n_sel)
    win_off = jnp.arange(WINDOW + NSA_QBLOCK) - WINDOW

    def block(qi):
        s0 = qi * NSA_QBLOCK
        t = s0 + jnp.arange(NSA_QBLOCK)
        qb = lax.dynamic_slice_in_dim(q, s0, NSA_QBLOCK, axis=1)
        gb = lax.dynamic_slice_in_dim(gates, s0, NSA_QBLOCK, axis=1)
        mask_c = (cmp_end[None, :] <= t[:, None])[None, :, None, None, :]
        pc = _masked_softmax(jnp.einsum('bqgrd,bcgd->bqgrc', qb, kcmp) * scale, mask_c)
        o_cmp = jnp.einsum('bqgrc,bcgd->bqgrd', pc, vcmp)
        local = t // SEL_BLOCK
        prio = jnp.where(sel_j[None, :] == local[:, None], 1e6, jnp.where(sel_j[None, :] > local[:, None], -jnp.inf, jnp.where(sel_j[None, :] == 0, 5e5, 0.0)))
        imp = jnp.einsum('bqgrc,cj->bqgj', pc, overlap) + prio[None, :, None, :]
        _, sel = lax.top_k(imp, SEL_TOPK)
        sel = jnp.swapaxes(sel, 1, 2)
        k_g = ksb[bi, gi, sel]
        v_g = vsb[bi, gi, sel]
        kpos = sel[..., None] * SEL_BLOCK + jnp.arange(SEL_BLOCK)
        mask_s = jnp.swapaxes(kpos <= t[None, None, :, None, None], 1, 2).reshape(b, NSA_QBLOCK, G, 1, SEL_TOPK * SEL_BLOCK)
        ss = jnp.einsum('bqgrd,bgqkld->bqgrkl', qb, k_g).reshape(b, NSA_QBLOCK, G, R, SEL_TOPK * SEL_BLOCK) * scale
        ps = _masked_softmax(ss, mask_s).reshape(b, NSA_QBLOCK, G, R, SEL_TOPK, SEL_BLOCK)
        o_slc = jnp.einsum('bqgrkl,bgqkld->bqgrd', ps, v_g)
        kwb = lax.dynamic_slice_in_dim(kwp, s0, WINDOW + NSA_QBLOCK, axis=1)
        vwb = lax.dynamic_slice_in_dim(vwp, s0, WINDOW + NSA_QBLOCK, axis=1)
        kpos_w = s0 + win_off
        mask_w = ((kpos_w[None, :] <= t[:, None]) & (kpos_w[None, :] > t[:, None] - WINDOW) & (kpos_w[None, :] >= 0))[None, :, None, None, :]
        pw = _masked_softmax(jnp.einsum('bqgrd,bkgd->bqgrk', qb, kwb) * scale, mask_w)
        o_win = jnp.einsum('bqgrk,bkgd->bqgrd', pw, vwb)
        return gb[..., 0:1] * o_cmp + gb[..., 1:2] * o_slc + gb[..., 2:3] * o_win

    out = lax.map(block, jnp.arange(s // NSA_QBLOCK))
    return jnp.moveaxis(out, 0, 1).reshape(b, s, NSA_WIDTH).astype(q.dtype)


def _hier_moe(h, w_rg, b_rg, w_re, b_re, w_g, w_u, w_d):
    b, s, d = h.shape
    t = h.reshape(b * s, d)
    tf = t.astype(jnp.float32)
    p_group = jax.nn.softmax(tf @ w_rg.astype(jnp.float32) + b_rg.astype(jnp.float32), axis=-1)
    pg, gsel = lax.top_k(p_group, 1)
    le = (tf @ w_re.astype(jnp.float32) + b_re.astype(jnp.float32)).reshape(-1, MOE_GROUPS, EXPERTS_PER_GROUP)
    le_g = jnp.einsum('tge,tg->te', le, jax.nn.one_hot(gsel[:, 0], MOE_GROUPS, dtype=jnp.float32))
    pe = jax.nn.softmax(le_g, axis=-1)
    pv, pi = lax.top_k(pe, EXPERT_TOPK)
    wts = pg * pv / jnp.sum(pv, axis=-1, keepdims=True)
    ids = gsel * EXPERTS_PER_GROUP + pi
    combine = jnp.sum(jax.nn.one_hot(ids, N_EXPERTS, dtype=jnp.float32) * wts[..., None], axis=1)
    out = jnp.zeros((b * s, d), jnp.float32)
    for e in range(N_EXPERTS):
        he = jax.nn.silu(t @ w_g[e]) * (t @ w_u[e])
        out = out + combine[:, e:e + 1] * (he @ w_d[e])
    return out.reshape(b, s, d).astype(h.dtype)


def _hybrid_layer(h, c, cos, sin, w_ada, b_ada, norm1_g, w_in, conv_w, conv_b, dt_bias, a_log, d_skip, ssm_norm_g, w_ssm_out, cmp_pe_k, cmp_w1_k, cmp_w2_k, cmp_pe_v, cmp_w1_v, cmp_w2_v, w_nsa_out, w_o, norm2_g, w_router_group, b_router_group, w_router_expert, b_router_expert, w_exp_gate, w_exp_up, w_exp_down):
    ada = jax.nn.silu(c) @ w_ada + b_ada
    shift1, scale1, gate1, shift2, scale2, gate2 = [a[:, None, :] for a in jnp.split(ada, 6, axis=-1)]
    hn = _rmsnorm(h, norm1_g) * (1.0 + scale1) + shift1
    offsets = [int(o) for o in np.cumsum(IN_SIZES)[:-1]]
    w_z, w_xbc, w_dt, w_q, w_kc, w_vc, w_ks, w_vs, w_kw, w_vw, w_gn, w_gs, w_ga = jnp.split(w_in, offsets, axis=1)
    y_ssm = _mamba2_mixer(hn @ w_z, hn @ w_xbc, hn @ w_dt, conv_w, conv_b, dt_bias, a_log, d_skip, ssm_norm_g) @ w_ssm_out
    y_nsa = _nsa_mixer(hn @ w_q, hn @ w_kc, hn @ w_vc, hn @ w_ks, hn @ w_vs, hn @ w_kw, hn @ w_vw, hn @ w_gn, cos, sin, cmp_pe_k, cmp_w1_k, cmp_w2_k, cmp_pe_v, cmp_w1_v, cmp_w2_v) @ w_nsa_out
    merged = jax.nn.sigmoid(hn @ w_gs) * y_ssm + jax.nn.sigmoid(hn @ w_ga) * y_nsa
    h = h + gate1 * (merged @ w_o)
    hn2 = _rmsnorm(h, norm2_g) * (1.0 + scale2) + shift2
    h = h + gate2 * _hier_moe(hn2, w_router_group, b_router_group, w_router_expert, b_router_expert, w_exp_gate, w_exp_up, w_exp_down)
    return h


def setup_inputs(seed: int = 0) -> dict:
    key = jax.random.key(seed)
    ks = jax.random.split(key, 40)
    f32 = jnp.float32

    def nrm(k, shape, fan_in):
        return jax.random.normal(k, shape, f32) * fan_in ** -0.5

    def gain(k, shape):
        return 1.0 + 0.05 * jax.random.normal(k, shape, f32)

    def small(k, shape, s=0.02):
        return s * jax.random.normal(k, shape, f32)

    L = DEPTH
    dt0 = jnp.exp(jax.random.uniform(ks[9], (L, SSM_HEADS), f32) * (math.log(0.1) - math.log(0.001)) + math.log(0.001))
    return {
        'x': jax.random.normal(ks[0], (BATCH, SEQ, D_MODEL), f32),
        'c': jax.random.normal(ks[1], (BATCH, D_MODEL), f32),
        'positions': jnp.arange(SEQ, dtype=jnp.int32)[None, :] + jax.random.randint(ks[2], (BATCH, 1), 0, 4096, dtype=jnp.int32),
        'w_ada': nrm(ks[3], (L, D_MODEL, 6 * D_MODEL), D_MODEL),
        'b_ada': small(ks[4], (L, 6 * D_MODEL)),
        'norm1_g': gain(ks[5], (L, D_MODEL)),
        'w_in': nrm(ks[6], (L, D_MODEL, IN_WIDTH), D_MODEL),
        'conv_w': nrm(ks[7], (L, SSM_CONV, SSM_CONV_DIM), SSM_CONV),
        'conv_b': small(ks[8], (L, SSM_CONV_DIM)),
        'dt_bias': dt0 + jnp.log(-jnp.expm1(-dt0)),
        'a_log': jnp.log(jax.random.uniform(ks[10], (L, SSM_HEADS), f32, minval=1.0, maxval=16.0)),
        'd_skip': 1.0 + 0.1 * jax.random.normal(ks[11], (L, SSM_HEADS), f32),
        'ssm_norm_g': gain(ks[12], (L, SSM_D_INNER)),
        'w_ssm_out': nrm(ks[13], (L, SSM_D_INNER, D_MODEL), SSM_D_INNER),
        'cmp_pe_k': small(ks[14], (L, CMP_BLOCK, NSA_HEAD_DIM)),
        'cmp_w1_k': nrm(ks[15], (L, CMP_BLOCK * NSA_HEAD_DIM, CMP_HIDDEN), CMP_BLOCK * NSA_HEAD_DIM),
        'cmp_w2_k': nrm(ks[16], (L, CMP_HIDDEN, NSA_HEAD_DIM), CMP_HIDDEN),
        'cmp_pe_v': small(ks[17], (L, CMP_BLOCK, NSA_HEAD_DIM)),
        'cmp_w1_v': nrm(ks[18], (L, CMP_BLOCK * NSA_HEAD_DIM, CMP_HIDDEN), CMP_BLOCK * NSA_HEAD_DIM),
        'cmp_w2_v': nrm(ks[19], (L, CMP_HIDDEN, NSA_HEAD_DIM), CMP_HIDDEN),
        'w_nsa_out': nrm(ks[20], (L, NSA_WIDTH, D_MODEL), NSA_WIDTH),
        'w_o': nrm(ks[21], (L, D_MODEL, D_MODEL), D_MODEL),
        'norm2_g': gain(ks[22], (L, D_MODEL)),
        'w_router_group': nrm(ks[23], (L, D_MODEL, MOE_GROUPS), D_MODEL),
        'b_router_group': small(ks[24], (L, MOE_GROUPS), 0.01),
        'w_router_expert': nrm(ks[25], (L, D_MODEL, N_EXPERTS), D_MODEL),
        'b_router_expert': small(ks[26], (L, N_EXPERTS), 0.01),
        'w_exp_gate': nrm(ks[27], (L, N_EXPERTS, D_MODEL, EXPERT_HIDDEN), D_MODEL),
        'w_exp_up': nrm(ks[28], (L, N_EXPERTS, D_MODEL, EXPERT_HIDDEN), D_MODEL),
        'w_exp_down': nrm(ks[29], (L, N_EXPERTS, EXPERT_HIDDEN, D_MODEL), EXPERT_HIDDEN),
        'final_g': gain(ks[30], (D_MODEL,)),
    }


def reference(x, c, positions, w_ada, b_ada, norm1_g, w_in, conv_w, conv_b, dt_bias, a_log, d_skip, ssm_norm_g, w_ssm_out, cmp_pe_k, cmp_w1_k, cmp_w2_k, cmp_pe_v, cmp_w1_v, cmp_w2_v, w_nsa_out, w_o, norm2_g, w_router_group, b_router_group, w_router_expert, b_router_expert, w_exp_gate, w_exp_up, w_exp_down, final_g):
    inv_freq = ROPE_THETA ** (-jnp.arange(0, NSA_HEAD_DIM, 2, dtype=jnp.float32) / NSA_HEAD_DIM)
    ang = positions.astype(jnp.float32)[..., None] * inv_freq
    cos, sin = jnp.cos(ang), jnp.sin(ang)
    h = x
    for l in range(DEPTH):
        h = _hybrid_layer(h, c, cos, sin, w_ada[l], b_ada[l], norm1_g[l], w_in[l], conv_w[l], conv_b[l], dt_bias[l], a_log[l], d_skip[l], ssm_norm_g[l], w_ssm_out[l], cmp_pe_k[l], cmp_w1_k[l], cmp_w2_k[l], cmp_pe_v[l], cmp_w1_v[l], cmp_w2_v[l], w_nsa_out[l], w_o[l], norm2_g[l], w_router_group[l], b_router_group[l], w_router_expert[l], b_router_expert[l], w_exp_gate[l], w_exp_up[l], w_exp_down[l])
    return _rmsnorm(h, final_g)
```

```python
import math
import numpy as np
from contextlib import ExitStack, contextmanager
import concourse.bass as bass
import concourse.mybir as mybir
from concourse.bass_utils import run_bass_kernel_spmd

F32 = mybir.dt.float32
BF16 = mybir.dt.bfloat16
I32 = mybir.dt.int32
AF = mybir.ActivationFunctionType
ALU = mybir.AluOpType
AX = mybir.AxisListType

NCORES = 8
D = 1024
SEQ = 2048
T = 512
NT = SEQ // T
EPS = 1e-6
OZ, OXBC, ODT, OQ, OKC, OVC, OKS, OVS, OKW, OVW, OGN, OGS, OGA = 0, 2048, 5120, 5152, 6176, 6304, 6432, 6560, 6688, 6816, 6944, 6992, 8016
SCALE = 64 ** -0.5
MAGIC = 12582912.0
TWO_PI = 2.0 * math.pi


def _cw_consts():
    c1 = np.float32(6.28125)
    r = np.float64(TWO_PI) - np.float64(c1)
    c2 = np.float32(np.round(r * 2 ** 20) / 2 ** 20)
    c3 = np.float32(r - np.float64(c2))
    return float(c1), float(c2), float(c3)


class KB:
    ENG = ('pe', 'act', 'dve', 'pool', 'sp')

    def __init__(self, nc):
        self.nc = nc
        self.root = ExitStack()
        self.cnt = {}
        self.known = {e: {} for e in self.ENG}
        self.sems = {}
        self.regs = {}
        self.residue = {}
        self.eng = {'pe': nc.tensor, 'act': nc.scalar, 'dve': nc.vector, 'pool': nc.gpsimd, 'sp': nc.sync}
        self.uid = 0
        self.ninst = 0
        self.taps = {}
        self.base = {}
        self.marks = []
        self.snap = {}
        self.out_dsems = []

    def semh(self, key):
        if key not in self.sems:
            self.sems[key] = self.root.enter_context(self.nc.semaphore('s%d' % len(self.sems)))
        return self.sems[key]

    def _newreg(self, nm):
        self.regs[nm] = [{}, dict(self.residue)]

    def sb(self, name, shape, dt, stack=None):
        self.uid += 1
        nm = '%s_%d' % (name, self.uid)
        st = stack if stack is not None else self.root
        t = st.enter_context(self.nc.sbuf_tensor(nm, list(shape), dt))
        self._newreg(nm)
        self.base[nm] = name
        if stack is not None:
            stack._names.append(nm)
        return t

    def psum(self, name, shape, dt):
        t = self.root.enter_context(self.nc.psum_tensor(name, list(shape), dt))
        self._newreg(name)
        return t

    def dram(self, name, shape, dt, kind):
        t = self.nc.dram_tensor(name, list(shape), dt, kind=kind)
        self._newreg(name)
        return t.ap()

    @contextmanager
    def scope(self):
        st = ExitStack()
        st._names = []
        try:
            yield st
        finally:
            for nm in st._names:
                w, r = self.regs.pop(nm)
                for d in (w, r):
                    for k, v in d.items():
                        if self.residue.get(k, 0) < v:
                            self.residue[k] = v
            st.close()

    def op(self, eng, fn, R, W, dsem=None, S=()):
        deps = {}
        strict = 0
        if eng in ('act', 'dve', 'pool') and dsem is None:
            for ap in R:
                v = self.regs[ap.name][0].get(eng, 0)
                if v > strict:
                    strict = v
        for ap in R:
            for k, v in self.regs[ap.name][0].items():
                if deps.get(k, 0) < v:
                    deps[k] = v
        for ap in W:
            w, r = self.regs[ap.name]
            for d in (w, r):
                for k, v in d.items():
                    if deps.get(k, 0) < v:
                        deps[k] = v
        kn = self.known[eng]
        E = self.eng[eng]
        if strict and kn.get(eng, 0) < strict:
            E.wait_ge(self.semh(eng), strict)
            kn[eng] = strict
        for k, v in sorted(deps.items(), key=lambda kv: -kv[1]):
            if k == eng:
                continue
            if kn.get(k, 0) < v:
                E.wait_ge(self.semh(k), v)
                kn[k] = v
                sn = self.snap.get((k, v))
                if sn:
                    for k2, v2 in sn.items():
                        if kn.get(k2, 0) < v2:
                            kn[k2] = v2
        key = dsem or eng
        inc = 16 if dsem else 1
        val = self.cnt.get(key, 0) + inc
        self.cnt[key] = val
        fn(E).then_inc(self.semh(key), inc)
        sn = dict(kn)
        if key == eng:
            sn[eng] = val - 1
        self.snap[(key, val)] = sn
        self.ninst += 1
        for ap in R:
            self.regs[ap.name][1][key] = val
        for ap in W:
            self.regs[ap.name] = [{key: val}, {}]

    def mm(self, out, lhsT, rhs, start=True, stop=True, extraR=()):
        self.op('pe', lambda e: e.matmul(out, lhsT=lhsT, rhs=rhs, start=start, stop=stop, skip_group_check=True),
                [lhsT, rhs] + list(extraR), [out])

    def tr(self, out, in_, ident):
        self.op('pe', lambda e: e.transpose(out, in_, ident), [in_, ident], [out])

    def act(self, out, in_, func, bias=None, scale=None, accum_out=None):
        R = [in_]
        S = []
        kw = {}
        if bias is not None:
            kw['bias'] = bias
            if not isinstance(bias, (int, float)):
                R.append(bias)
                S.append(bias)
        if scale is not None:
            kw['scale'] = scale
            if not isinstance(scale, (int, float)):
                R.append(scale)
                S.append(scale)
        W = [out]
        if accum_out is not None:
            kw['accum_out'] = accum_out
            W.append(accum_out)
        self.op('act', lambda e: e.activation(out=out, in_=in_, func=func, **kw), R, W, S=S)

    def tt(self, eng, out, in0, in1, op):
        self.op(eng, lambda e: e.tensor_tensor(out=out, in0=in0, in1=in1, op=op), [in0, in1], [out])

    def ts(self, eng, out, in0, s1, op0, s2=None, op1=None):
        S = [s for s in (s1, s2) if s is not None and not isinstance(s, (int, float))]
        R = [in0] + S
        if op1 is None:
            self.op(eng, lambda e: e.tensor_scalar(out=out, in0=in0, scalar1=s1, scalar2=None, op0=op0), R, [out], S=S)
        else:
            self.op(eng, lambda e: e.tensor_scalar(out=out, in0=in0, scalar1=s1, scalar2=s2, op0=op0, op1=op1), R, [out], S=S)

    def stt(self, out, in0, scalar, in1, op0, op1):
        S = [] if isinstance(scalar, (int, float)) else [scalar]
        R = [in0, in1] + S
        self.op('dve', lambda e: e.scalar_tensor_tensor(out=out, in0=in0, scalar=scalar, in1=in1, op0=op0, op1=op1), R, [out], S=S)

    def copy(self, eng, out, in_):
        if eng == 'act':
            self.op('act', lambda e: e.copy(out=out, in_=in_), [in_], [out])
        else:
            self.op(eng, lambda e: e.tensor_copy(out=out, in_=in_), [in_], [out])

    def memset(self, eng, ap, val):
        self.op(eng, lambda e: e.memset(ap, val), [], [ap])

    def dma(self, out, in_, eng='sp', dsem=None):
        if dsem is None:
            dsem = 'd_' + self.base.get(out.name, out.name)
        self.op(eng, lambda e: e.dma_start(out=out, in_=in_), [in_], [out], dsem=dsem)
        return dsem

    def tap(self, name, ap, dt=F32):
        shp = list(ap.shape)
        d = self.dram('tap_' + name, shp, dt, "ExternalOutput")
        ds = self.dma(d, ap, dsem='d_tap_' + name)
        self.out_dsems.append(ds)
        self.taps[name] = shp

    def mark(self, label):
        self.marks.append((label, dict(self.cnt)))

    def finish(self):
        E = self.eng['sp']
        for ds in self.out_dsems:
            v = self.cnt[ds]
            if self.known['sp'].get(ds, 0) < v:
                E.wait_ge(self.semh(ds), v)
                self.known['sp'][ds] = v
        self.root.close()


def bc(ap, shape):
    return ap.to_broadcast(list(shape))


def _swap64(cols):
    c = np.asarray(cols).reshape(-1, 64)
    return np.concatenate([c[:, 32:], c[:, :32]], axis=1).reshape(-1)


def _fm_chunk_cols():
    ssd = [OXBC + i * 128 + np.arange(128) for i in range(24)]
    ssd.append(np.tile(ODT + np.arange(32), 4))
    nsa = []
    for i in range(8):
        c = OQ + i * 128 + np.arange(128)
        nsa += [c, _swap64(c)]
    for off in (OKS, OKW):
        for g in range(2):
            c = np.tile(off + g * 64 + np.arange(64), 2)
            nsa += [c, _swap64(c)]
    c = OKC + np.arange(128)
    nsa += [c, _swap64(c)]
    nsa.append(OVC + np.arange(128))
    gate = [OGS + i * 128 + np.arange(128) for i in range(8)] + [OGA + i * 128 + np.arange(128) for i in range(8)]
    return ssd, nsa, gate


N_SSD, N_NSA, N_GATE = 25, 27, 16
NFC = N_SSD + N_NSA + N_GATE


def _kmaj(w):
    K, N = w.shape
    return np.ascontiguousarray(w.reshape(K // 128, 128, N).transpose(1, 0, 2))


def _cols(v, n):
    return np.ascontiguousarray(np.asarray(v).reshape(n, 128).T)


def prep_shared(inp):
    f = np.float32
    w_in = np.asarray(inp['w_in'][0], f)
    ssd, nsa, gate = _fm_chunk_cols()
    wfm = np.stack([_kmaj(w_in[:, c]) for c in (ssd + nsa + gate)], 0)
    sh = {}
    sh['wfm'] = wfm.reshape(NFC, 128, 1024)
    sh['wtz'] = np.stack([_kmaj(w_in[:, OZ + g * 512: OZ + (g + 1) * 512]) for g in range(4)], 0).reshape(4, 128, 4096)
    tv = np.concatenate([OVS + np.arange(128), OVW + np.arange(128), OGN + np.arange(48)])
    sh['wtv'] = _kmaj(w_in[:, tv]).reshape(1, 128, 8 * 304)
    wada = np.asarray(inp['w_ada'][0], f)
    sh['wada'] = np.stack([_kmaj(wada[:, i * 128:(i + 1) * 128]) for i in range(48)], 0).reshape(48, 128, 1024)
    wso = np.asarray(inp['w_ssm_out'][0], f)
    sh['wso'] = np.stack([_kmaj(wso[:, m * 128:(m + 1) * 128]) for m in range(8)], 0).reshape(8, 128, 2048)
    wno = np.asarray(inp['w_nsa_out'][0], f)
    sh['wno'] = np.stack([_kmaj(wno[:, m * 128:(m + 1) * 128]) for m in range(8)], 0).reshape(8, 128, 1024)
    wo = np.asarray(inp['w_o'][0], f)
    sh['wo'] = np.stack([_kmaj(wo[:, m * 128:(m + 1) * 128]) for m in range(8)], 0).reshape(8, 128, 1024)
    for nm, key in (('w1k', 'cmp_w1_k'), ('w1v', 'cmp_w1_v')):
        w1 = np.asarray(inp[key][0], f).reshape(32, 64, 256).transpose(1, 0, 2)
        sh[nm] = np.ascontiguousarray(np.concatenate([w1, w1], 0)).reshape(1, 128, 32 * 256)
    w2k = np.asarray(inp['cmp_w2_k'][0], f)
    sh['w2k'] = _kmaj(np.concatenate([w2k, w2k], 1)).reshape(1, 128, 2 * 128)
    sh['w2v'] = _kmaj(np.asarray(inp['cmp_w2_v'][0], f)).reshape(1, 128, 2 * 64)
    sh['pekT'] = np.ascontiguousarray(np.asarray(inp['cmp_pe_k'][0], f).T)
    sh['pevT'] = np.ascontiguousarray(np.asarray(inp['cmp_pe_v'][0], f).T)
    sh['wg'] = np.stack([_kmaj(np.asarray(inp['w_exp_gate'][0, e], f)) for e in range(32)], 0).reshape(32, 128, 2048)
    sh['wu'] = np.stack([_kmaj(np.asarray(inp['w_exp_up'][0, e], f)) for e in range(32)], 0).reshape(32, 128, 2048)
    sh['wd'] = np.stack([_kmaj(np.asarray(inp['w_exp_down'][0, e], f)) for e in range(32)], 0).reshape(32, 128, 2048)
    wr = np.concatenate([np.asarray(inp['w_router_group'][0], f), np.asarray(inp['w_router_expert'][0], f)], 1)
    sh['wr'] = _kmaj(wr).reshape(128, 8 * 36)
    br = np.concatenate([np.asarray(inp['b_router_group'][0], f), np.asarray(inp['b_router_expert'][0], f)])
    sh['br'] = np.ascontiguousarray(np.broadcast_to(br[None, :], (128, 36)))
    vec = np.zeros((128, 160), f)
    vec[:, 0:48] = _cols(inp['b_ada'][0], 48)
    vec[:, 48:56] = _cols(inp['norm1_g'][0], 8)
    vec[:, 56:64] = _cols(inp['norm2_g'][0], 8)
    vec[:, 64:72] = _cols(inp['final_g'], 8)
    vec[:, 72:96] = _cols(inp['conv_b'][0], 24)
    vec[0:32, 96] = np.asarray(inp['dt_bias'][0], f)
    vec[0:32, 97] = np.asarray(inp['a_log'][0], f)
    sh['vec'] = vec
    cw = np.asarray(inp['conv_w'][0], f)
    sh['cw'] = np.ascontiguousarray(cw.reshape(4, 24, 128).transpose(2, 1, 0)).reshape(128, 96)
    sh['dsk'] = np.ascontiguousarray(np.broadcast_to(np.asarray(inp['d_skip'][0], f)[None, :], (128, 32)))
    sh['sng'] = np.ascontiguousarray(np.broadcast_to(np.asarray(inp['ssm_norm_g'][0], f)[None, :], (128, 2048)))
    p = np.arange(128)
    sh['ident'] = np.eye(128, dtype=f)
    tl = np.arange(512) % 128
    sh['neg4'] = np.where(tl[None, :] < p[:, None], f(-30000.0), f(0.0)).astype(f)
    mw = np.zeros((128, 8, 512), f)
    for r in range(-4, 4):
        krel = 128 * r + p[:, None]
        q = np.arange(512)[None, :]
        mw[:, r + 4, :] = ((krel <= q) & (krel > q - 512)).astype(f)
    sh['maskw'] = mw.reshape(128, 8 * 512)
    c = p - 1
    tt_ = np.arange(2048)
    sh['maskc'] = ((c[:, None] >= 0) & (16 * c[:, None] + 31 <= tt_[None, :])).astype(f)
    j = np.arange(32)
    ovl = ((16 * c[:, None] < 64 * j[None, :] + 64) & (16 * c[:, None] + 32 > 64 * j[None, :]) & (c[:, None] >= 0)).astype(f)
    sh['ovl'] = ovl
    pr = np.zeros((128, 16, 32), f)
    for qb in range(16):
        local = (128 * qb + p) // 64
        pr[:, qb, :] = np.where(j[None, :] == local[:, None], f(1e6),
                                np.where(j[None, :] > local[:, None], f(-1e30),
                                         np.where(j[None, :] == 0, f(5e5), f(0.0))))
    sh['prio'] = pr.reshape(128, 512)
    invf = (10000.0 ** (-np.arange(0, 64, 2, dtype=np.float32) / 64)).astype(f)
    rc = np.zeros((128, 2), f)
    rc[:, 0] = invf[p % 32]
    rc[:, 1] = np.where((p % 64) < 32, -1.0, 1.0)
    sh['ropec'] = rc
    return sh


SCRATCH = ['wfm', 'wtz', 'wtv', 'wso', 'wno', 'wo', 'w1k', 'w1v', 'w2k', 'w2v', 'wg', 'wu', 'wd']


def build(nseq, ntiles=None, dbg=None, stop_phase=99, dbg_tile=0, dbg_chunk=0, nsa_stop=99):
    dbg = dbg or set()
    ntiles = NT if ntiles is None else ntiles
    ntok = nseq * SEQ
    nc = bass.Bass("TRN2", target_bir_lowering=False)
    kb = KB(nc)
    C1, C2, C3 = _cw_consts()
    PI_LO = 3.1415925

    def ext_in(name, shape, dt=F32):
        return kb.dram(name, shape, dt, "ExternalInput")

    xT = ext_in('xT', [8, 128, ntok])
    cT = ext_in('cT', [128, 8 * nseq])
    pos = ext_in('pos', [nseq, SEQ], I32)
    shapes = {'wfm': [NFC, 128, 1024], 'wtz': [4, 128, 4096], 'wtv': [1, 128, 2432], 'wso': [8, 128, 2048],
              'wno': [8, 128, 1024], 'wo': [8, 128, 1024], 'w1k': [1, 128, 8192], 'w1v': [1, 128, 8192],
              'w2k': [1, 128, 256], 'w2v': [1, 128, 128], 'wg': [32, 128, 2048], 'wu': [32, 128, 2048],
              'wd': [32, 128, 2048]}
    src = {k: ext_in(k, v) for k, v in shapes.items()}
    scr = {k: kb.dram('b_' + k, v, BF16, "Internal") for k, v in shapes.items()}
    wada_d = ext_in('wada', [48, 128, 1024])
    small = {k: ext_in(k, s) for k, s in (('pekT', [64, 32]), ('pevT', [64, 32]), ('wr', [128, 288]), ('br', [128, 36]),
                                          ('vec', [128, 160]), ('cw', [128, 96]), ('dsk', [128, 32]), ('sng', [128, 2048]),
                                          ('ident', [128, 128]), ('neg4', [128, 512]), ('maskw', [128, 4096]),
                                          ('maskc', [128, 2048]), ('ovl', [128, 32]), ('prio', [128, 512]), ('ropec', [128, 2]))}
    outT = kb.dram('outT', [8, 128, ntok], F32, "ExternalOutput")

    PS = [kb.psum('ps%d' % i, [128, 512], F32) for i in range(8)]

    def psb(i):
        return PS[i][:, :].bitcast(BF16)

    identF = kb.sb('identF', [128, 128], F32)
    identB = kb.sb('identB', [128, 128], BF16)
    onesF = kb.sb('onesF', [128, 128], F32)
    NEG4 = kb.sb('neg4', [128, 512], BF16)
    maskw_d = kb.dram('b_maskw', [128, 4096], BF16, "Internal")
    maskc_d = kb.dram('b_maskc', [128, 2048], BF16, "Internal")
    ROPEC = kb.sb('ropec', [128, 2], F32)
    VEC = kb.sb('vec', [128, 160], F32)
    CW = kb.sb('cw', [128, 24, 4], F32)
    DSK = kb.sb('dsk', [128, 32], F32)
    SNG = kb.sb('sng', [128, 2048], F32)
    WR = kb.sb('wr', [128, 8, 36], F32)
    BR = kb.sb('br', [128, 36], F32)
    OVL = kb.sb('ovl', [128, 32], F32)
    W2K = kb.sb('w2k', [128, 2, 128], BF16)
    W2V = kb.sb('w2v', [128, 2, 64], BF16)
    PEB = kb.sb('peb', [128, 4], F32)
    ADA = kb.sb('ada', [128, 48, nseq], F32)
    GM1 = kb.sb('gm1', [128, 8, nseq], F32)
    GM2 = kb.sb('gm2', [128, 8, nseq], F32)
    AN = kb.sb('an', [32, 1], F32)
    EPSC = kb.sb('epsc', [128, 1], F32)
    ONEC = kb.sb('onec', [128, 1], F32)

    kb.dma(identF[:], small['ident'])
    kb.dma(ROPEC[:], small['ropec'])
    kb.dma(VEC[:], small['vec'])
    kb.dma(CW[:].rearrange("p a b -> p (a b)"), small['cw'])
    kb.dma(DSK[:], small['dsk'])
    kb.dma(SNG[:], small['sng'])
    kb.dma(WR[:].rearrange("p a b -> p (a b)"), small['wr'])
    kb.dma(BR[:], small['br'])
    kb.dma(OVL[:], small['ovl'])
    kb.copy('dve', identB[:], identF[:])
    kb.memset('dve', onesF[:], 1.0)
    kb.memset('dve', EPSC[:], EPS)
    kb.memset('dve', ONEC[:], 1.0)
    with kb.scope() as sc:
        stg = kb.sb('stgc', [128, 512], F32, sc)
        kb.dma(stg[:], small['neg4'])
        kb.copy('dve', NEG4[:], stg[:])
        for dst, nm, n in ((maskw_d, 'maskw', 4096), (maskc_d, 'maskc', 2048)):
            for c0 in range(0, n, 2048):
                stb = kb.sb('stgb_%s_%d' % (nm, c0), [128, 2048], BF16, sc)
                kb.dma(stb[:], small[nm][:, c0:c0 + 2048], eng='pool')
                kb.dma(dst[:, c0:c0 + 2048], stb[:])

    with kb.scope() as sc:
        stage = [kb.sb('stage%d' % i, [128, 8192], BF16, sc) for i in range(2)]
        si = 0
        for k in SCRATCH:
            G, _, R = shapes[k]
            gp = max(1, 8192 // R)
            for g0 in range(0, G, gp):
                g1 = min(G, g0 + gp)
                st = stage[si % 2]
                si += 1
                v = st[:, 0:(g1 - g0) * R].rearrange("p (g r) -> p g r", g=g1 - g0)
                kb.dma(v, src[k][g0:g1].rearrange("g p r -> p g r"), eng='pool')
                kb.dma(scr[k][g0:g1].rearrange("g p r -> p g r"), v, eng='sp')
    kb.dma(W2K[:].rearrange("p a b -> p (a b)"), scr['w2k'][0])
    kb.dma(W2V[:].rearrange("p a b -> p (a b)"), scr['w2v'][0])

    kb.act(AN[:], VEC[0:32, 97:98], AF.Exp)
    kb.ts('dve', AN[:], AN[:], -1.0, ALU.mult)
    with kb.scope() as sc:
        CTs = kb.sb('cts', [128, 8, nseq], F32, sc)
        kb.dma(CTs[:].rearrange("p a b -> p (a b)"), cT)
        SC_ = kb.sb('sc', [128, 8, nseq], F32, sc)
        kb.act(SC_[:], CTs[:], AF.Silu)
        wslots = [kb.sb('wadas%d' % i, [128, 8, 128], F32, sc) for i in range(3)]
        for fch in range(48):
            ws = wslots[fch % 3]
            kb.dma(ws[:].rearrange("p a b -> p (a b)"), wada_d[fch])
            bank = PS[fch % 2]
            for kc in range(8):
                kb.mm(bank[:, 0:nseq], ws[:, kc, :], SC_[:, kc, :], start=(kc == 0), stop=(kc == 7))
            kb.ts('dve', ADA[:, fch, :], bank[:, 0:nseq], VEC[:, fch:fch + 1], ALU.add)
        for b in range(nseq):
            kb.stt(GM1[:, :, b], ADA[:, 8:16, b], 1.0, VEC[:, 48:56], ALU.add, ALU.mult)
            kb.stt(GM2[:, :, b], ADA[:, 32:40, b], 1.0, VEC[:, 56:64], ALU.add, ALU.mult)
        for kv, (wn, pn) in enumerate((('w1k', 'pekT'), ('w1v', 'pevT'))):
            W1 = kb.sb('w1s%d' % kv, [128, 32, 256], BF16, sc)
            kb.dma(W1[:].rearrange("p a b -> p (a b)"), scr[wn][0])
            pef = kb.sb('pef%d' % kv, [64, 32], F32, sc)
            kb.dma(pef[:], small[pn])
            peb = kb.sb('pebf', [64, 32], BF16, sc)
            kb.copy('dve', peb[:], pef[:])
            for m in range(2):
                for l in range(32):
                    kb.mm(PS[2][:, m:m + 1], W1[0:64, l, m * 128:(m + 1) * 128], peb[:, l:l + 1], start=(l == 0), stop=(l == 31))
            kb.copy('dve', PEB[:, kv * 2:kv * 2 + 2], PS[2][:, 0:2])
    if 'ada' in dbg:
        kb.tap('ada', ADA[:].rearrange("p a b -> p (a b)"))
        kb.tap('peb', PEB[:])

    KC_T = kb.sb('kct', [128, 16 + SEQ], BF16)
    VC_T = kb.sb('vct', [128, 16 + SEQ], BF16)
    KS = [kb.sb('ks%d' % g, [128, SEQ], BF16) for g in range(2)]
    KW = [kb.sb('kw%d' % g, [128, SEQ], BF16) for g in range(2)]
    VS_AUG = kb.sb('vsaug', [128, 16, 2, 65], BF16)
    VW_AUG = kb.sb('vwaug', [128, 16, 2, 65], BF16)
    KCMP = [kb.sb('kcmp%d' % g, [128, 128], BF16) for g in range(2)]
    VCMP = kb.sb('vcmp', [128, 2, 97], BF16)
    STATE = kb.sb('state', [128, 2048], F32)
    STATE_B = kb.sb('stateb', [128, 2048], BF16)
    HALO = kb.sb('halo', [128, 24, 3], BF16)
    H1AVP = kb.sb('h1avp', [128, 4, 128], BF16)
    HN = kb.sb('hn', [128, 8, T], BF16)
    YSSM = kb.sb('yssm', [128, 8, T], BF16)
    WF = [kb.sb('wf%d' % i, [128, 4, 8, 128], BF16) for i in range(2)]
    WM = [kb.sb('wm%d' % i, [128, 16, 128], BF16) for i in range(2)]

    kb.memset('pool', VS_AUG[:].rearrange("p a b c -> p (a b c)"), 1.0)
    kb.memset('pool', VW_AUG[:].rearrange("p a b c -> p (a b c)"), 1.0)

    wf_ctr = [0]

    def load_wf(c0, n):
        s = WF[wf_ctr[0] % 2]
        wf_ctr[0] += 1
        kb.dma(s[:, 0:n].rearrange("p c k m -> p c (k m)"), scr['wfm'][c0:c0 + n].rearrange("c p r -> p c r"))
        return s

    wm_ctr = [0]

    def load_wm(name, m, nk):
        s = WM[wm_ctr[0] % 2]
        wm_ctr[0] += 1
        kb.dma(s[:, 0:nk].rearrange("p k m -> p (k m)"), scr[name][m])
        return s

    def rmsnorm_rstd(src_t, sc, bankidx=0):
        SQ = [kb.sb('sq%d' % i, [128, T], F32, sc) for i in range(2)]
        for kc in range(8):
            kb.act(SQ[kc % 2][:], src_t[:, kc, :], AF.Square)
            kb.mm(PS[bankidx][:, :], onesF[:, :], SQ[kc % 2][:], start=(kc == 0), stop=(kc == 7))
        RT = kb.sb('rt', [128, T], F32, sc)
        kb.act(RT[:], PS[bankidx][:, :], AF.Sqrt, bias=EPSC[:, 0:1], scale=1.0 / D)
        RSTD = kb.sb('rstd', [128, T], F32, sc)
        kb.op('dve', lambda e: e.reciprocal(out=RSTD[:], in_=RT[:]), [RT[:]], [RSTD[:]])
        return RSTD

    def seq_init(b):
        kb.memset('pool', KC_T[:, 0:16], 0.0)
        kb.memset('pool', VC_T[:, 0:16], 0.0)
        for g in range(2):
            kb.memset('pool', KCMP[g][:], 0.0)
        kb.memset('pool', VCMP[:].rearrange("p a b -> p (a b)"), 0.0)
        for g in range(2):
            kb.memset('pool', VCMP[:, g, 64:65], 1.0)
            kb.copy('pool', VCMP[:, g, 65:97], OVL[:])
        kb.memset('pool', STATE[:], 0.0)
        kb.memset('pool', STATE_B[:], 0.0)
        kb.memset('pool', HALO[:].rearrange("p a b -> p (a b)"), 0.0)

    def phase1_norm1(b, tt, tok0):
        with kb.scope() as sc:
            XT = kb.sb('xt', [128, 8, T], F32, sc)
            kb.dma(XT[:], xT[:, :, tok0:tok0 + T].rearrange("k p t -> p k t"))
            RSTD = rmsnorm_rstd(XT, sc)
            TMP = [kb.sb('n1tmp%d' % i, [128, T], F32, sc) for i in range(2)]
            for kc in range(8):
                tm = TMP[kc % 2]
                kb.tt('dve', tm[:], XT[:, kc, :], RSTD[:], ALU.mult)
                kb.act(HN[:, kc, :], tm[:], AF.Identity, bias=ADA[:, 0 + kc, b:b + 1], scale=GM1[:, kc, b:b + 1])
        if 'hn' in dbg and b == 0 and tt == dbg_tile:
            kb.tap('hn', HN[:].rearrange("p a b -> p (a b)"), BF16)

    def phase2_ssd_proj(b, tt, sc):
        XBC = kb.sb('xbc', [128, 24, T], BF16, sc)
        ZS = kb.sb('zs', [128, 4, 2048], BF16, sc)
        DT_T = kb.sb('dtT', [128, T], F32, sc)
        kb.memset('pool', DT_T[:, :], 0.0)
        DTA_T = kb.sb('dtaT', [32, T], F32, sc)
        with kb.scope() as s2:
            U = [kb.sb('u%d' % i, [128, T + 3], BF16, s2) for i in range(4)]
            DG = [kb.sb('dg%d' % i, [128, 4, 128], BF16, s2) for i in range(4)]
            for i in range(N_SSD):
                if i % 4 == 0:
                    wf = load_wf(i, min(4, N_SSD - i))
                bankA = PS[i % 4]
                for kc in range(8):
                    kb.mm(bankA[:, :], wf[:, i % 4, kc, :], HN[:, kc, :], start=(kc == 0), stop=(kc == 7))
                if i < 24:
                    u = U[i % 4]
                    dg = DG[i % 4]
                    kb.copy('act', u[:, 3:T + 3], bankA[:, :])
                    kb.copy('dve', u[:, 0:3], HALO[:, i, :])
                    kb.copy('dve', HALO[:, i, :], u[:, T:T + 3])
                    for k in range(4):
                        kb.ts('dve', dg[:, k, :], identB[:, :], CW[:, i, k:k + 1], ALU.mult)
                ip = i - 1
                if 0 <= ip < 24:
                    up, dgp = U[ip % 4], DG[ip % 4]
                    bankB = PS[4 + ip % 2]
                    for k in range(4):
                        kb.mm(bankB[:, :], dgp[:, k, :], up[:, k:k + T], start=(k == 0), stop=(k == 3))
                    kb.act(XBC[:, ip, :], bankB[:, :], AF.Silu, bias=VEC[:, 72 + ip:73 + ip])
                if i >= 24:
                    XD = kb.sb('xd', [32, T], F32, s2)
                    MX = kb.sb('mx', [32, T], F32, s2)
                    NA = kb.sb('na', [32, T], F32, s2)
                    kb.ts('dve', XD[:], bankA[0:32, :], VEC[0:32, 96:97], ALU.add)
                    kb.ts('dve', MX[:], XD[:], 0.0, ALU.max)
                    kb.stt(NA[:], MX[:], -2.0, XD[:], ALU.mult, ALU.add)
                    kb.act(NA[:], NA[:], AF.Exp)
                    kb.act(NA[:], NA[:], AF.Ln, bias=ONEC[0:32, 0:1])
                    kb.tt('dve', DT_T[0:32, :], MX[:], NA[:], ALU.add)
                    kb.ts('dve', DTA_T[:], DT_T[0:32, :], AN[:, 0:1], ALU.mult)
            WTZ = [kb.sb('wtz%d' % i, [128, 8, 512], BF16, s2) for i in range(2)]
            for g in range(4):
                wz = WTZ[g % 2]
                kb.dma(wz[:].rearrange("p k n -> p (k n)"), scr['wtz'][g])
                for c in range(4):
                    bank = PS[6 + c % 2]
                    for kc in range(8):
                        kb.mm(bank[:, :], HN[:, kc, c * 128:(c + 1) * 128], wz[:, kc, :], start=(kc == 0), stop=(kc == 7))
                    kb.act(ZS[:, c, g * 512:(g + 1) * 512], bank[:, :], AF.Silu)
        if b == 0 and tt == dbg_tile:
            if 'xbc' in dbg:
                kb.tap('xbc', XBC[:].rearrange("p a b -> p (a b)"), BF16)
            if 'dt' in dbg:
                kb.tap('dt', DT_T[0:32, :])
            if 'zs' in dbg:
                kb.tap('zs', ZS[:].rearrange("p a b -> p (a b)"), BF16)
        return XBC, ZS, DT_T, DTA_T

    def phase3_ssd(b, tt, sc, XBC, ZS, DT_T, DTA_T):
        YNT = kb.sb('ynt', [128, 16, T], BF16, sc)
        with kb.scope() as s3:
            XS = kb.sb('xs', [128, 2048], BF16, s3)
            XDT = kb.sb('xdt', [128, 2048], BF16, s3)
            XW = kb.sb('xw', [128, 2048], BF16, s3)
            BTOK = kb.sb('btok', [128, 4, 128], BF16, s3)
            ACUMT = kb.sb('acumT', [128, T], F32, s3)
            kb.memset('pool', ACUMT[:, :], 0.0)
            ATOK = kb.sb('atok', [128, 32], F32, s3)
            DTOK = kb.sb('dtok', [128, 32], F32, s3)
            NEGA = kb.sb('nega', [128, 32], F32, s3)
            EA = kb.sb('ea', [128, 32], F32, s3)
            D1 = kb.sb('d1', [128, 32], F32, s3)
            WEND = kb.sb('wend', [128, 32], F32, s3)
            DECS = kb.sb('decs', [128, 32], F32, s3)
            DIAGA = kb.sb('diaga', [128, 32], F32, s3)
            kb.memset('pool', DIAGA[:, :], 0.0)
            CBT = kb.sb('cbt', [128, 4, 128], F32, s3)
            LT = [kb.sb('lt%d' % i, [128, 4, 128], BF16, s3) for i in range(3)]
            MT = [kb.sb('mt%d' % i, [128, 4, 128], BF16, s3) for i in range(3)]
            YT = kb.sb('yt', [128, 2048], F32, s3)
            TMPA = [kb.sb('s3tmp%d' % i, [128, 512], F32, s3) for i in range(2)]
            YN = XDT
            SS = kb.sb('ss', [128, 4], F32, s3)
            RS = kb.sb('rs', [128, 4], F32, s3)
            qc = [0]
            for c in range(4):
                cs = slice(c * 128, (c + 1) * 128)
                for i in range(16):
                    kb.tr(psb(i // 8)[:, (i % 8) * 128:(i % 8 + 1) * 128], XBC[:, i, cs], identB[:, :])
                kb.copy('act', XS[:, 0:1024], psb(0)[:, :])
                kb.copy('dve', XS[:, 1024:2048], psb(1)[:, :])
                for g in range(4):
                    kb.tr(psb(0)[:, g * 128:(g + 1) * 128], XBC[:, 16 + g, cs], identB[:, :])
                kb.copy('act', BTOK[:].rearrange("p a b -> p (a b)"), psb(0)[:, 0:512])
                kb.op('dve', lambda e: e.tensor_tensor_scan(out=ACUMT[0:32, cs], data0=onesF[0:32, 0:128], data1=DTA_T[:, cs],
                                                            initial=0.0, op0=ALU.mult, op1=ALU.add),
                      [onesF[:, :], DTA_T[:, :]], [ACUMT[:, :]])
                kb.tr(PS[1][:, 0:128], ACUMT[:, cs], identF[:, :])
                kb.tr(PS[1][:, 128:256], DT_T[:, cs], identF[:, :])
                kb.copy('dve', ATOK[:], PS[1][:, 0:32])
                kb.copy('dve', DTOK[:], PS[1][:, 128:160])
                kb.ts('dve', NEGA[:], ATOK[:], -1.0, ALU.mult)
                kb.act(EA[:], ATOK[:], AF.Exp)
                kb.ts('dve', DIAGA[0:32, :], identF[0:32, 0:32], ACUMT[0:32, c * 128 + 127:c * 128 + 128], ALU.mult)
                kb.mm(PS[1][:, 256:288], onesF[:, 0:128], DIAGA[:, :])
                kb.tt('dve', D1[:], PS[1][:, 256:288], ATOK[:], ALU.subtract)
                kb.act(D1[:], D1[:], AF.Exp)
                kb.tt('dve', WEND[:], D1[:], DTOK[:], ALU.mult)
                kb.act(DECS[:], PS[1][:, 256:288], AF.Exp)
                kb.tt('dve', XDT[:].rearrange("p (h d) -> p h d", h=32), XS[:].rearrange("p (h d) -> p h d", h=32),
                      bc(DTOK[:].unsqueeze(2), [128, 32, 64]), ALU.mult)
                kb.tt('pool', XW[:].rearrange("p (h d) -> p h d", h=32), XS[:].rearrange("p (h d) -> p h d", h=32),
                      bc(WEND[:].unsqueeze(2), [128, 32, 64]), ALU.mult)
                for g in range(4):
                    kb.mm(PS[6][:, g * 128:(g + 1) * 128], XBC[:, 16 + g, cs], XBC[:, 20 + g, cs])
                kb.copy('act', CBT[:].rearrange("p a b -> p (a b)"), PS[6][:, :])
                SEGB = [PS[2], PS[3], PS[6]]

                def quad_unit(q):
                    g, q2 = q // 2, q % 2
                    gs_ = slice(g * 512, (g + 1) * 512)
                    st = {}

                    def s1():
                        sl = qc[0] % 3
                        qc[0] += 1
                        lt, mt, bankS = LT[sl], MT[sl], SEGB[sl]
                        st['mt'] = mt
                        for j in range(4):
                            kb.mm(bankS[:, j * 128:(j + 1) * 128], bc(identF[:, 4 * q + j:4 * q + j + 1], [128, 128]), ACUMT[:, cs],
                                  start=(j == 0), stop=False)
                        kb.mm(bankS[:, :], identB[:, :], NEG4[:, :], start=False, stop=True)
                        for j in range(4):
                            h = 4 * q + j
                            kb.act(lt[:, j, :], bankS[:, j * 128:(j + 1) * 128], AF.Exp, bias=NEGA[:, h:h + 1])
                        kb.tt('dve', mt[:], lt[:], bc(CBT[:, g, :].unsqueeze(1), [128, 4, 128]), ALU.mult)

                    def s2():
                        mt = st['mt']
                        bankY = PS[4 + g % 2]
                        for j in range(4):
                            h = 4 * q + j
                            kb.mm(bankY[:, (4 * q2 + j) * 64:(4 * q2 + j + 1) * 64], mt[:, j, :], XDT[:, h * 64:(h + 1) * 64])
                        if q2 == 1:
                            kb.mm(PS[7][:, :], XBC[:, 20 + g, cs], STATE_B[:, gs_])
                            tm = TMPA[g % 2]
                            kb.tt('dve', tm[:].rearrange("p (h d) -> p h d", h=8), PS[7][:, :].rearrange("p (h d) -> p h d", h=8),
                                  bc(EA[:, 8 * g:8 * g + 8].unsqueeze(2), [128, 8, 64]), ALU.mult)
                            kb.tt('dve', YT[:, gs_], bankY[:, :], tm[:], ALU.add)
                            kb.mm(PS[0][:, :], BTOK[:, g, :], XW[:, gs_])
                            tm2 = TMPA[(g + 1) % 2]
                            kb.tt('pool', tm2[:].rearrange("p (h d) -> p h d", h=8), STATE[:, gs_].rearrange("p (h d) -> p h d", h=8),
                                  bc(DECS[:, 8 * g:8 * g + 8].unsqueeze(2), [128, 8, 64]), ALU.mult)
                            kb.tt('dve', STATE[:, gs_], tm2[:], PS[0][:, :], ALU.add)
                            kb.copy('pool', STATE_B[:, gs_], STATE[:, gs_])
                    return (s1, s2)

                units = [quad_unit(q) for q in range(8)]
                for i in range(2):
                    units[i][0]()
                for i in range(8):
                    units[i][1]()
                    if i + 2 < 8:
                        units[i + 2][0]()
                for g in range(4):
                    gs_ = slice(g * 512, (g + 1) * 512)
                    tm = TMPA[g % 2]
                    kb.tt('dve', tm[:].rearrange("p (h d) -> p h d", h=8), XS[:, gs_].rearrange("p (h d) -> p h d", h=8),
                          bc(DSK[:, 8 * g:8 * g + 8].unsqueeze(2), [128, 8, 64]), ALU.mult)
                    kb.tt('dve', YT[:, gs_], YT[:, gs_], tm[:], ALU.add)
                kb.tt('dve', YT[:], YT[:], ZS[:, c, :], ALU.mult)
                kb.act(XW[:], YT[:], AF.Square)
                kb.op('dve', lambda e: e.tensor_reduce(out=SS[:], in_=XW[:].rearrange("p (g d) -> p g d", g=4), axis=AX.X, op=ALU.add),
                      [XW[:]], [SS[:]])
                kb.act(RS[:], SS[:], AF.Sqrt, bias=EPSC[:, 0:1], scale=1.0 / 512)
                kb.op('dve', lambda e: e.reciprocal(out=RS[:], in_=RS[:]), [RS[:]], [RS[:]])
                for g in range(4):
                    gs_ = slice(g * 512, (g + 1) * 512)
                    kb.stt(YN[:, gs_], YT[:, gs_], RS[:, g:g + 1], SNG[:, gs_], ALU.mult, ALU.mult)
                if b == 0 and tt == dbg_tile and c == dbg_chunk:
                    if 'yt' in dbg:
                        kb.tap('yt', YT[:])
                    if 'yn' in dbg:
                        kb.tap('yn', YN[:], BF16)
                    if 'atok' in dbg:
                        kb.tap('atok', ATOK[:])
                for i in range(16):
                    kb.tr(psb(i // 8)[:, (i % 8) * 128:(i % 8 + 1) * 128], YN[:, i * 128:(i + 1) * 128], identB[:, :])
                kb.copy('act', YNT[:, 0:8, cs], psb(0)[:, :].rearrange("p (a b) -> p a b", a=8))
                kb.copy('act', YNT[:, 8:16, cs], psb(1)[:, :].rearrange("p (a b) -> p a b", a=8))
        for m in range(8):
            w = load_wm('wso', m, 16)
            bank = PS[m % 4]
            for kc in range(16):
                kb.mm(bank[:, :], w[:, kc, :], YNT[:, kc, :], start=(kc == 0), stop=(kc == 15))
            kb.copy('act', YSSM[:, m, :], bank[:, :])
        if 'yssm' in dbg and b == 0 and tt == dbg_tile:
            kb.tap('yssm', YSSM[:].rearrange("p a b -> p (a b)"), BF16)

    def rope_tables(b, tt, st):
        t0 = tt * T
        COS = kb.sb('cos', [128, T], F32, st)
        SINS = kb.sb('sins', [128, T], F32, st)
        with kb.scope() as sr:
            POSI = kb.sb('posi', [128, T], I32, sr)
            kb.dma(POSI[:], pos[b:b + 1, t0:t0 + T].partition_broadcast(128))
            ANG = kb.sb('ang', [128, T], F32, sr)
            TK = kb.sb('tk', [128, T], F32, sr)
            RR = kb.sb('rr', [128, T], F32, sr)
            kb.copy('dve', ANG[:], POSI[:])
            kb.ts('dve', ANG[:], ANG[:], ROPEC[:, 0:1], ALU.mult)
            for which in (0, 1):
                if which == 0:
                    kb.ts('dve', TK[:], ANG[:], 1.0 / TWO_PI, ALU.mult, MAGIC, ALU.add)
                else:
                    kb.ts('dve', TK[:], ANG[:], 1.0 / TWO_PI, ALU.mult, 0.25, ALU.add)
                    kb.ts('dve', TK[:], TK[:], MAGIC, ALU.add)
                kb.ts('dve', TK[:], TK[:], -MAGIC, ALU.add)
                kb.stt(RR[:], TK[:], -C1, ANG[:], ALU.mult, ALU.add)
                kb.stt(RR[:], TK[:], -C2, RR[:], ALU.mult, ALU.add)
                kb.stt(RR[:], TK[:], -C3, RR[:], ALU.mult, ALU.add)
                if which == 1:
                    kb.ts('dve', RR[:], RR[:], math.pi / 2, ALU.add)
                kb.ts('dve', RR[:], RR[:], PI_LO, ALU.min, -PI_LO, ALU.max)
                if which == 0:
                    kb.act(SINS[:], RR[:], AF.Sin, scale=ROPEC[:, 1:2])
                else:
                    kb.act(COS[:], RR[:], AF.Sin)
        return COS, SINS

    def phase4_nsa(b, tt, sc, tok0, COS, SINS):
        t0 = tt * T
        ATTT = kb.sb('attT', [128, 8, T], BF16, sc)
        with kb.scope() as s4:
            QX = kb.sb('qx', [128, 2, 8, T], BF16, s4)
            kb.memset('pool', QX[64:128, 0].rearrange("p a b -> p (a b)"), 0.0)
            kb.memset('pool', QX[0:64, 1].rearrange("p a b -> p (a b)"), 0.0)
            GATE = kb.sb('gate', [128, 4, 48], F32, s4)
            if b == 0 and tt == dbg_tile and 'cos' in dbg:
                kb.tap('cos', COS[:])
                kb.tap('sins', SINS[:])
            if nsa_stop <= 1:
                return ATTT
            sw_scope = kb.scope()
            sw = sw_scope.__enter__()
            W1S = []
            for kv, wn in enumerate(('w1k', 'w1v')):
                W1_ = kb.sb('w1_%d' % kv, [128, 32, 256], BF16, sw)
                kb.dma(W1_[:].rearrange("p a b -> p (a b)"), scr[wn][0])
                W1S.append(W1_)
            with kb.scope() as sp_:
                T1 = [kb.sb('rt1_%d' % i, [128, T], F32, sp_) for i in range(2)]
                T2 = [kb.sb('rt2_%d' % i, [128, T], F32, sp_) for i in range(2)]
                dests = [None for i in range(8)] + [KS[0][:, t0:t0 + T], KS[1][:, t0:t0 + T], KW[0][:, t0:t0 + T],
                                                          KW[1][:, t0:t0 + T], KC_T[:, 16 + t0:16 + t0 + T]]
                for pi in range(14):
                    ci = 2 * pi
                    for cc in (ci, ci + 1):
                        if cc < N_NSA and cc % 4 == 0:
                            wf = load_wf(N_SSD + cc, min(4, N_NSA - cc))
                        if cc < N_NSA:
                            bank = PS[(cc % 4)]
                            for kc in range(8):
                                kb.mm(bank[:, :], wf[:, cc % 4, kc, :], HN[:, kc, :], start=(kc == 0), stop=(kc == 7))
                    if pi < 13:
                        bA, bB = PS[ci % 4], PS[(ci + 1) % 4]
                        t1, t2 = T1[pi % 2], T2[pi % 2]
                        kb.tt('dve', t1[:], bA[:, :], COS[:], ALU.mult)
                        kb.tt('dve', t2[:], bB[:, :], SINS[:], ALU.mult)
                        if pi < 8:
                            kb.tt('pool', QX[0:64, 0, pi, :], t1[0:64, :], t2[0:64, :], ALU.add)
                            kb.tt('pool', QX[64:128, 1, pi, :], t1[64:128, :], t2[64:128, :], ALU.add)
                        else:
                            kb.tt('pool', dests[pi], t1[:], t2[:], ALU.add)
                    else:
                        kb.copy('act', VC_T[:, 16 + t0:16 + t0 + T], PS[ci % 4][:, :])
                WTV = kb.sb('wtv', [128, 8, 304], BF16, sp_)
                kb.dma(WTV[:].rearrange("p k n -> p (k n)"), scr['wtv'][0])
                for c in range(4):
                    bank = PS[4 + c % 2]
                    kbk = 4 * tt + c
                    for kc in range(8):
                        kb.mm(bank[:, 0:304], HN[:, kc, c * 128:(c + 1) * 128], WTV[:, kc, :], start=(kc == 0), stop=(kc == 7))
                    kb.copy('act', VS_AUG[:, kbk, :, 0:64], bank[:, 0:128].rearrange("p (g d) -> p g d", g=2))
                    kb.copy('act', VW_AUG[:, kbk, :, 0:64], bank[:, 128:256].rearrange("p (g d) -> p g d", g=2))
                    kb.act(GATE[:, c, :], bank[:, 256:304], AF.Sigmoid)
            if b == 0 and tt == dbg_tile and 'q' in dbg:
                kb.tap('kw0', KW[0][:, t0:t0 + T], BF16)
                kb.tap('gate', GATE[:].rearrange("p a b -> p (a b)"))
            if nsa_stop <= 2:
                return ATTT
            with kb.scope() as scp:
                H1A = kb.sb('h1a', [128, 4, 32], BF16, scp)
                for kv, (wn, SRC) in enumerate((('w1k', KC_T), ('w1v', VC_T))):
                    W1 = W1S[kv]
                    for g in range(2):
                        for m in range(2):
                            col = (g * 2 + m) * 32
                            for l in range(32):
                                kb.mm(PS[6 - g][:, col:col + 32], W1[64 * g:64 * g + 64, l, m * 128:(m + 1) * 128],
                                      SRC[64 * g:64 * g + 64, t0 + l:t0 + l + 497:16], start=(l == 0), stop=(l == 31))
                    for g in range(2):
                        for m in range(2):
                            col = (g * 2 + m) * 32
                            dst = H1A[:, g * 2 + m, :] if kv == 0 else H1AVP[:, g * 2 + m, 32 * tt:32 * tt + 32]
                            kb.act(dst, PS[6 - g][:, col:col + 32], AF.Silu, bias=PEB[:, kv * 2 + m:kv * 2 + m + 1])
                    if kv == 0:
                        for g in range(2):
                            for m in range(2):
                                kb.mm(PS[7][:, g * 32:(g + 1) * 32], W2K[:, m, :], H1A[:, g * 2 + m, :], start=(m == 0), stop=(m == 1))
                        for g in range(2):
                            kb.copy('act', KCMP[g][:, 32 * tt:32 * tt + 32], PS[7][:, g * 32:(g + 1) * 32])
                    else:
                        p0 = 32 * tt if tt < 3 else 64
                        p1 = 32 * tt + 32
                        for g in range(2):
                            for m in range(2):
                                kb.mm(PS[7][p0:p1, 64 + g * 64:128 + g * 64], H1AVP[:, g * 2 + m, p0:p1], W2V[:, m, :],
                                      start=(m == 0), stop=(m == 1))
                        kb.copy('act', VCMP[p0:p1, :, 0:64], PS[7][p0:p1, 64:192].rearrange("p (g d) -> p g d", g=2))
            sw_scope.__exit__(None, None, None)
            if b == 0 and tt == dbg_tile and 'kcmp' in dbg:
                kb.tap('kcmp', KCMP[0][:], BF16)
                kb.tap('vcmp', VCMP[:].rearrange("p a b -> p (a b)"), BF16)
            if nsa_stop <= 3:
                return ATTT
            MASKW = kb.sb('maskw', [128, 8, 512], BF16, s4)
            kb.dma(MASKW[:].rearrange("p a b -> p (a b)"), maskw_d)
            MASKC = kb.sb('maskc', [128, T], BF16, s4)
            kb.dma(MASKC[:], maskc_d[:, t0:t0 + T])
            PRIO = kb.sb('prio', [128, 4, 32], F32, s4)
            kb.dma(PRIO[:].rearrange("p a b -> p (a b)"), small['prio'][:, 4 * tt * 32:(4 * tt + 4) * 32])
            ATT = kb.sb('att', [128, 4, 1024], BF16, s4)
            ATTB = kb.sb('attb', [128, 4, 1024], BF16, s4)
            IMP = [kb.sb('imp%d' % g, [128, 4, 32], F32, s4) for g in range(2)]
            PT = [kb.sb('pt%d' % i, [128, T], BF16, s4) for i in range(5)]
            NRM = kb.sb('nrm', [128, 4, 97], F32, s4)
            RD = kb.sb('rd', [128, 4], F32, s4)
            CF = kb.sb('cf', [128, 4], F32, s4)
            TMO = kb.sb('tmo', [128, 4, 64], F32, s4)
            ptc = [0]

            def oview(bank, w):
                return bank[:, :].rearrange("p (q c) -> p q c", q=4)[:, :, 0:w]

            OB = [PS[4], PS[5], PS[7]]
            SB5 = [PS[0], PS[1], PS[2], PS[3], PS[6]]
            hctr = [0]

            def run_units(units, D=3):
                n = len(units)
                for i in range(min(D, n)):
                    units[i][0]()
                for i in range(n):
                    units[i][1]()
                    if i + D < n:
                        units[i + D][0]()

            def cmp_unit(hc, e2):
                g = hc // 4
                h = 2 * hc + e2
                ps_ = slice(64 * e2, 64 * e2 + 64)
                st = {}

                def s1():
                    st['bankS'] = SB5[ptc[0] % 5]
                    st['pt'] = PT[ptc[0] % 5]
                    ptc[0] += 1
                    kb.mm(st['bankS'][:, :], KCMP[g][:, :], QX[:, e2, hc, :])
                    kb.act(st['pt'][:], st['bankS'][:, :], AF.Exp, scale=SCALE)
                    kb.tt('dve', st['pt'][:], st['pt'][:], MASKC[:, :], ALU.mult)

                def s2():
                    pt = st['pt']
                    bankO = OB[hctr[0] % 3]
                    hctr[0] += 1
                    for qb in range(4):
                        kb.mm(bankO[:, qb * 128:qb * 128 + 97], pt[:, qb * 128:(qb + 1) * 128], VCMP[:, g, :], start=(qb == 0), stop=(qb == 3))
                    ov = oview(bankO, 97)
                    kb.ts('dve', RD[:], ov[:, :, 64], 1e-30, ALU.max)
                    kb.op('dve', lambda e: e.reciprocal(out=RD[:], in_=RD[:]), [RD[:]], [RD[:]])
                    kb.tt('dve', NRM[:], ov, bc(RD[:].unsqueeze(2), [128, 4, 97]), ALU.mult)
                    if h % 8 == 0:
                        kb.copy('pool', IMP[g][:], NRM[:, :, 65:97])
                    else:
                        kb.tt('pool', IMP[g][:], IMP[g][:], NRM[:, :, 65:97], ALU.add)
                    kb.tt('dve', ATT[:, :, h * 64:(h + 1) * 64], NRM[:, :, 0:64], bc(GATE[:, :, 3 * h:3 * h + 1], [128, 4, 64]), ALU.mult)
                return (s1, s2)

            run_units([cmp_unit(hc, e2) for hc in range(8) for e2 in range(2)])
            if b == 0 and tt == dbg_tile and 'ocmp' in dbg:
                kb.tap('ocmp', ATT[:].rearrange("p a b -> p (a b)"), BF16)
                kb.tap('imp', IMP[0][:].rearrange("p a b -> p (a b)"))
            if nsa_stop <= 4:
                return ATTT
            nkb = 4 * tt + 4
            SELT = [kb.sb('selT%d' % g, [32, T], BF16, s4) for g in range(2)]
            MEXP = kb.sb('mexp', [128, nkb, 2, T], BF16, s4)
            IMPP = kb.sb('impp', [128, 32], F32, s4)
            MAX8 = kb.sb('max8', [128, 8], F32, s4)
            SEL = kb.sb('sel', [128, 32], BF16, s4)
            for g in range(2):
                for qb in range(4):
                    kb.tt('dve', IMPP[:], IMP[g][:, qb, :], PRIO[:, qb, :], ALU.add)
                    kb.op('dve', lambda e: e.max(out=MAX8[:], in_=IMPP[:]), [IMPP[:]], [MAX8[:]])
                    kb.ts('dve', SEL[:], IMPP[:], MAX8[:, 3:4], ALU.is_ge)
                    kb.tr(psb(6)[0:32, qb * 128:(qb + 1) * 128], SEL[:], identB[:, :])
                kb.copy('act', SELT[g][:], psb(6)[0:32, 0:512])
            if b == 0 and tt == dbg_tile and 'selt' in dbg:
                kb.tap('selt', SELT[0][:], BF16)
            for kbk in range(nkb):
                for g in range(2):
                    for hf in range(2):
                        kb.mm(PS[7][64 * hf:64 * hf + 64, :], bc(identB[0:32, 2 * kbk + hf:2 * kbk + hf + 1], [32, 64]), SELT[g][:, :])
                    kb.copy('act', MEXP[:, kbk, g, :], PS[7][:, :])
                    if kbk >= 4 * tt:
                        kb.tt('pool', MEXP[:, kbk, g, :], MEXP[:, kbk, g, :], MASKW[:, 4 + kbk - 4 * tt, :], ALU.mult)

            if nsa_stop <= 5:
                return ATTT
            def branch_units(KK, VAUG, gate_idx, kb_lo, use_mexp, last):
                units = []
                for hc in range(8):
                    g = hc // 4
                    per = []
                    for e2 in range(2):
                        h = 2 * hc + e2
                        ps_ = slice(64 * e2, 64 * e2 + 64)
                        hstate = {}
                        kbs = list(range(kb_lo, nkb))
                        per.append([br_unit(KK, VAUG, gate_idx, use_mexp, last, hc, g, h, ps_, kbk, ki == 0, ki == len(kbs) - 1, hstate)
                                    for ki, kbk in enumerate(kbs)])
                    for ua, ub in zip(per[0], per[1]):
                        units += [ua, ub]
                return units

            def br_unit(KK, VAUG, gate_idx, use_mexp, last, hc, g, h, ps_, kbk, is_first, is_last, hstate):
                e2x = h % 2
                r = kbk - 4 * tt
                q0 = max(0, r)
                q1 = 3 if use_mexp else min(3, r + 4)
                cols = slice(q0 * 128, (q1 + 1) * 128)
                st = {}

                def s1():
                    st['bankS'] = SB5[ptc[0] % 5]
                    st['pt'] = PT[ptc[0] % 5]
                    ptc[0] += 1
                    bankS, pt = st['bankS'], st['pt']
                    kb.mm(bankS[:, cols], KK[g][:, kbk * 128:(kbk + 1) * 128], QX[:, e2x, hc, cols])
                    kb.act(pt[:, cols], bankS[:, cols], AF.Exp, scale=SCALE)
                    if use_mexp:
                        kb.tt('dve', pt[:, cols], pt[:, cols], MEXP[:, kbk, g, cols], ALU.mult)
                    else:
                        mq = q0 if r >= 0 else q1
                        mc = slice(mq * 128, (mq + 1) * 128)
                        kb.tt('dve', pt[:, mc], pt[:, mc], MASKW[:, r + 4, mc], ALU.mult)

                def s2():
                    pt = st['pt']
                    if is_first:
                        hstate['bankO'] = OB[hctr[0] % 3]
                        hctr[0] += 1
                    bankO = hstate['bankO']
                    for qi, qb in enumerate(range(q0, q1 + 1)):
                        kb.mm(bankO[:, qb * 128:qb * 128 + 65], pt[:, qb * 128:(qb + 1) * 128], VAUG[:, kbk, g, :],
                              start=(is_first and qi == 0), stop=False)
                    if is_last:
                        ov = oview(bankO, 65)
                        kb.ts('dve', RD[:], ov[:, :, 64], 1e-30, ALU.max)
                        kb.op('dve', lambda e: e.reciprocal(out=RD[:], in_=RD[:]), [RD[:]], [RD[:]])
                        kb.tt('dve', CF[:], RD[:], GATE[:, :, 3 * h + gate_idx], ALU.mult)
                        kb.tt('dve', TMO[:], ov[:, :, 0:64], bc(CF[:].unsqueeze(2), [128, 4, 64]), ALU.mult)
                        if last:
                            kb.tt('pool', ATTB[:, :, h * 64:(h + 1) * 64], ATT[:, :, h * 64:(h + 1) * 64], TMO[:], ALU.add)
                        else:
                            kb.tt('pool', ATT[:, :, h * 64:(h + 1) * 64], ATT[:, :, h * 64:(h + 1) * 64], TMO[:], ALU.add)
                return (s1, s2)

            run_units(branch_units(KW, VW_AUG, 2, max(0, 4 * tt - 4), False, False)
                      + branch_units(KS, VS_AUG, 1, 0, True, True), D=4)
            if b == 0 and tt == dbg_tile and 'attb' in dbg:
                kb.tap('attb', ATTB[:].rearrange("p a b -> p (a b)"), BF16)
            for qb in range(4):
                for i in range(8):
                    kb.tr(psb(6)[:, i * 128:(i + 1) * 128], ATTB[:, qb, i * 128:(i + 1) * 128], identB[:, :])
                kb.copy('act', ATTT[:, :, qb * 128:(qb + 1) * 128], psb(6)[:, :].rearrange("p (a b) -> p a b", a=8))
        return ATTT

    def phase5_merge(b, tt, sc, tok0, ATTT):
        H = kb.sb('h', [128, 8, T], F32, sc)
        kb.dma(H[:], xT[:, :, tok0:tok0 + T].rearrange("k p t -> p k t"))
        with kb.scope() as s5:
            SG = kb.sb('sg', [128, 16, T], BF16, s5)
            MRG = kb.sb('mrg', [128, 8, T], BF16, s5)
            for i in range(N_GATE):
                if i % 4 == 0:
                    wf = load_wf(N_SSD + N_NSA + i, 4)
                bank = PS[i % 4]
                for kc in range(8):
                    kb.mm(bank[:, :], wf[:, i % 4, kc, :], HN[:, kc, :], start=(kc == 0), stop=(kc == 7))
                kb.act(SG[:, i, :], bank[:, :], AF.Sigmoid)
            T1 = [kb.sb('m1_%d' % i, [128, T], F32, s5) for i in range(2)]
            T2 = [kb.sb('m2_%d' % i, [128, T], F32, s5) for i in range(2)]
            for m in range(8):
                w = load_wm('wno', m, 8)
                bank = PS[4 + m % 4]
                for kc in range(8):
                    kb.mm(bank[:, :], w[:, kc, :], ATTT[:, kc, :], start=(kc == 0), stop=(kc == 7))
                t1, t2 = T1[m % 2], T2[m % 2]
                kb.tt('dve', t1[:], bank[:, :], SG[:, 8 + m, :], ALU.mult)
                kb.tt('dve', t2[:], YSSM[:, m, :], SG[:, m, :], ALU.mult)
                kb.tt('pool', MRG[:, m, :], t1[:], t2[:], ALU.add)
            if b == 0 and tt == dbg_tile and 'mrg' in dbg:
                kb.tap('mrg', MRG[:].rearrange("p a b -> p (a b)"), BF16)
            for m in range(8):
                w = load_wm('wo', m, 8)
                bank = PS[m % 4]
                for kc in range(8):
                    kb.mm(bank[:, :], w[:, kc, :], MRG[:, kc, :], start=(kc == 0), stop=(kc == 7))
                kb.stt(H[:, m, :], bank[:, :], ADA[:, 16 + m, b:b + 1], H[:, m, :], ALU.mult, ALU.add)
        if b == 0 and tt == dbg_tile and 'h1' in dbg:
            kb.tap('h1', H[:].rearrange("p a b -> p (a b)"))
        return H

    def phase6_moe(b, tt, sc, tok0, H):
        with kb.scope() as s6:
            COMBT = kb.sb('combT', [128, T], F32, s6)
            kb.memset('pool', COMBT[:, :], 0.0)
            s6r = kb.scope()
            s6r_st = s6r.__enter__()
            HN2F = kb.sb('hn2f', [128, 8, T], F32, s6r_st)
            with kb.scope() as sn:
                RSTD = rmsnorm_rstd(H, sn)
                TMP = [kb.sb('n2tmp%d' % i, [128, T], F32, sn) for i in range(2)]
                for kc in range(8):
                    tm = TMP[kc % 2]
                    kb.tt('dve', tm[:], H[:, kc, :], RSTD[:], ALU.mult)
                    kb.act(HN2F[:, kc, :], tm[:], AF.Identity, bias=ADA[:, 24 + kc, b:b + 1], scale=GM2[:, kc, b:b + 1])
                    kb.copy('pool', HN[:, kc, :], HN2F[:, kc, :])
            if b == 0 and tt == dbg_tile and 'hn2' in dbg:
                kb.tap('hn2', HN2F[:].rearrange("p a b -> p (a b)"))
            with kb.scope() as sr:
                LG = kb.sb('lg', [128, 36], F32, sr)
                GMX = kb.sb('gmx', [128, 1], F32, sr)
                NGM = kb.sb('ngm', [128, 1], F32, sr)
                GOH = kb.sb('goh', [128, 4], F32, sr)
                EJ = kb.sb('ej', [128, 4], F32, sr)
                SUMG = kb.sb('sumg', [128, 1], F32, sr)
                PG = kb.sb('pg', [128, 1], F32, sr)
                LE8 = kb.sb('le8', [128, 8], F32, sr)
                M8 = kb.sb('m8', [128, 8], F32, sr)
                DD = kb.sb('dd', [128, 1], F32, sr)
                W1_ = kb.sb('w1_', [128, 1], F32, sr)
                A1 = kb.sb('a1', [128, 1], F32, sr)
                A2 = kb.sb('a2', [128, 1], F32, sr)
                C8 = kb.sb('c8', [128, 8], F32, sr)
                C8b = kb.sb('c8b', [128, 8], F32, sr)
                COMB = kb.sb('comb', [128, 32], F32, sr)
                for c in range(4):
                    for kc in range(8):
                        kb.mm(PS[1][:, 0:36], HN2F[:, kc, c * 128:(c + 1) * 128], WR[:, kc, :], start=(kc == 0), stop=(kc == 7))
                    kb.tt('dve', LG[:], PS[1][:, 0:36], BR[:], ALU.add)
                    kb.op('dve', lambda e: e.tensor_reduce(out=GMX[:], in_=LG[:, 0:4], axis=AX.X, op=ALU.max), [LG[:]], [GMX[:]])
                    kb.ts('dve', GOH[:], LG[:, 0:4], GMX[:, 0:1], ALU.is_equal)
                    kb.ts('dve', NGM[:], GMX[:], -1.0, ALU.mult)
                    kb.act(EJ[:], LG[:, 0:4], AF.Exp, bias=NGM[:, 0:1])
                    kb.op('dve', lambda e: e.tensor_reduce(out=SUMG[:], in_=EJ[:], axis=AX.X, op=ALU.add), [EJ[:]], [SUMG[:]])
                    kb.op('dve', lambda e: e.reciprocal(out=PG[:], in_=SUMG[:]), [SUMG[:]], [PG[:]])
                    kb.ts('dve', LE8[:], LG[:, 4:12], GOH[:, 0:1], ALU.mult)
                    for g in range(1, 4):
                        kb.stt(LE8[:], LG[:, 4 + 8 * g:12 + 8 * g], GOH[:, g:g + 1], LE8[:], ALU.mult, ALU.add)
                    kb.op('dve', lambda e: e.max(out=M8[:], in_=LE8[:]), [LE8[:]], [M8[:]])
                    kb.tt('dve', DD[:], M8[:, 1:2], M8[:, 0:1], ALU.subtract)
                    kb.act(DD[:], DD[:], AF.Exp)
                    kb.ts('dve', W1_[:], DD[:], 1.0, ALU.add)
                    kb.op('dve', lambda e: e.reciprocal(out=W1_[:], in_=W1_[:]), [W1_[:]], [W1_[:]])
                    kb.tt('dve', A1[:], W1_[:], PG[:], ALU.mult)
                    kb.tt('dve', A2[:], A1[:], DD[:], ALU.mult)
                    kb.ts('dve', C8[:], LE8[:], M8[:, 0:1], ALU.is_equal, A1[:, 0:1], ALU.mult)
                    kb.ts('dve', C8b[:], LE8[:], M8[:, 1:2], ALU.is_equal, A2[:, 0:1], ALU.mult)
                    kb.tt('dve', C8[:], C8[:], C8b[:], ALU.add)
                    for g in range(4):
                        kb.ts('dve', COMB[:, 8 * g:8 * g + 8], C8[:], GOH[:, g:g + 1], ALU.mult)
                    kb.tr(PS[2][0:32, 0:128], COMB[:], identF[:, :])
                    kb.copy('dve', COMBT[0:32, c * 128:(c + 1) * 128], PS[2][0:32, 0:128])
            s6r.__exit__(None, None, None)
            if b == 0 and tt == dbg_tile and 'comb' in dbg:
                kb.tap('comb', COMBT[0:32, :])
            ACC = kb.sb('acc', [128, 8, T], F32, s6)
            with kb.scope() as se:
                HE = kb.sb('he', [128, 16, T], BF16, se)
                WG = [kb.sb('wg%d' % i, [128, 8, 256], BF16, se) for i in range(2)]
                WU = [kb.sb('wu%d' % i, [128, 8, 256], BF16, se) for i in range(2)]
                WD = [kb.sb('wd%d' % i, [128, 2, 1024], BF16, se) for i in range(2)]
                CBE = [kb.sb('cbe%d' % i, [128, T], F32, se) for i in range(2)]
                SGT = [kb.sb('sgt%d' % i, [128, T], F32, se) for i in range(2)]
                TU = [kb.sb('tu%d' % i, [128, T], F32, se) for i in range(2)]
                for grp in range(4):
                    for e8 in range(8):
                        e = grp * 8 + e8
                        wg, wu = WG[e % 2], WU[e % 2]
                        kb.dma(wg[:].rearrange("p k n -> p (k n)"), scr['wg'][e])
                        kb.dma(wu[:].rearrange("p k n -> p (k n)"), scr['wu'][e])
                        cbe = CBE[e % 2]
                        kb.mm(PS[4][:, :], bc(identF[:, e:e + 1], [128, 128]), COMBT[:, :])
                        kb.copy('act', cbe[:], PS[4][:, :])
                        for j in range(2):
                            bG, bU = PS[2 * j], PS[2 * j + 1]
                            for kc in range(8):
                                kb.mm(bG[:, :], wg[:, kc, j * 128:(j + 1) * 128], HN[:, kc, :], start=(kc == 0), stop=(kc == 7))
                            for kc in range(8):
                                kb.mm(bU[:, :], wu[:, kc, j * 128:(j + 1) * 128], HN[:, kc, :], start=(kc == 0), stop=(kc == 7))
                            sg, tu = SGT[j], TU[j]
                            kb.act(sg[:], bG[:, :], AF.Silu)
                            kb.tt('dve', tu[:], bU[:, :], sg[:], ALU.mult)
                            kb.tt('pool', HE[:, e8 * 2 + j, :], tu[:], cbe[:], ALU.mult)
                    for e8 in range(8):
                        e = grp * 8 + e8
                        wd = WD[e % 2]
                        kb.dma(wd[:].rearrange("p k n -> p (k n)"), scr['wd'][e])
                        for m in range(8):
                            for j in range(2):
                                kb.mm(PS[m][:, :], wd[:, j, m * 128:(m + 1) * 128], HE[:, e8 * 2 + j, :],
                                      start=(e8 == 0 and j == 0), stop=(e8 == 7 and j == 1))
                    for m in range(8):
                        if grp == 0:
                            kb.copy('act', ACC[:, m, :], PS[m][:, :])
                        elif grp < 3:
                            kb.tt('dve', ACC[:, m, :], ACC[:, m, :], PS[m][:, :], ALU.add)
                        else:
                            kb.tt('dve', ACC[:, m, :], ACC[:, m, :], PS[m][:, :], ALU.add)
                            kb.stt(H[:, m, :], ACC[:, m, :], ADA[:, 40 + m, b:b + 1], H[:, m, :], ALU.mult, ALU.add)
            if b == 0 and tt == dbg_tile and 'h2' in dbg:
                kb.tap('h2', H[:].rearrange("p a b -> p (a b)"))
            with kb.scope() as sn:
                RSTD = rmsnorm_rstd(H, sn)
                OUT = ACC
                for kc in range(8):
                    kb.stt(OUT[:, kc, :], H[:, kc, :], VEC[:, 64 + kc:65 + kc], RSTD[:], ALU.mult, ALU.mult)
                ds = kb.dma(outT[:, :, tok0:tok0 + T].rearrange("k p t -> p k t"), OUT[:], dsem='d_out')
                if ds not in kb.out_dsems:
                    kb.out_dsems.append(ds)

    for b in range(nseq):
        kb.mark('seqinit')
        seq_init(b)
        for tt in range(ntiles):
            tok0 = b * SEQ + tt * T
            kb.mark('p1')
            phase1_norm1(b, tt, tok0)
            if stop_phase <= 1:
                continue
            tile_scope = kb.scope()
            st_tile = tile_scope.__enter__()
            COS, SINS = rope_tables(b, tt, st_tile)
            with kb.scope() as sc:
                kb.mark('p2')
                XBC, ZS, DT_T, DTA_T = phase2_ssd_proj(b, tt, sc)
                if stop_phase <= 2:
                    continue
                kb.mark('p3')
                phase3_ssd(b, tt, sc, XBC, ZS, DT_T, DTA_T)
            if stop_phase <= 3:
                continue
            with kb.scope() as sc:
                kb.mark('p4')
                ATTT = phase4_nsa(b, tt, sc, tok0, COS, SINS)
                if stop_phase <= 4:
                    continue
                kb.mark('p5')
                H = phase5_merge(b, tt, sc, tok0, ATTT)
                if stop_phase <= 5:
                    continue
                kb.mark('p6')
                phase6_moe(b, tt, sc, tok0, H)
            tile_scope.__exit__(None, None, None)
    kb.mark('end')
    kb.finish()
    return nc, kb


def core_inputs(inp, shared, b0, nseq):
    x = np.asarray(inp['x'][b0:b0 + nseq], np.float32).reshape(nseq * SEQ, D)
    m = dict(shared)
    m['xT'] = np.ascontiguousarray(x.T).reshape(8, 128, nseq * SEQ)
    c = np.asarray(inp['c'][b0:b0 + nseq], np.float32)
    m['cT'] = np.ascontiguousarray(c.reshape(nseq, 8, 128).transpose(2, 1, 0)).reshape(128, 8 * nseq)
    m['pos'] = np.ascontiguousarray(np.asarray(inp['positions'][b0:b0 + nseq], np.int32))
    return m


_CACHE = {}


def kernel(**inputs):
    B = inputs['x'].shape[0]
    nseq = B // NCORES
    if 'nc' not in _CACHE:
        _CACHE['nc'] = build(nseq)[0]
    nc = _CACHE['nc']
    shared = prep_shared(inputs)
    in_maps = [core_inputs(inputs, shared, i * nseq, nseq) for i in range(NCORES)]
    res = run_bass_kernel_spmd(nc, in_maps, core_ids=list(range(NCORES)))
    out = np.empty((B, SEQ, D), np.float32)
    for i in range(NCORES):
        o = np.asarray(res.results[i]['outT']).reshape(D, nseq * SEQ)
        out[i * nseq:(i + 1) * nseq] = o.T.reshape(nseq, SEQ, D)
    return out
```

```python
import math
import numpy as np
from contextlib import ExitStack, contextmanager
import concourse.bass as bass
import concourse.mybir as mybir
from concourse.bass_utils import run_bass_kernel_spmd

F32 = mybir.dt.float32
BF16 = mybir.dt.bfloat16
I32 = mybir.dt.int32
AF = mybir.ActivationFunctionType
ALU = mybir.AluOpType
AX = mybir.AxisListType

NCORES = 8
D = 1024
SEQ = 2048
T = 512
NT = SEQ // T
EPS = 1e-6
OZ, OXBC, ODT, OQ, OKC, OVC, OKS, OVS, OKW, OVW, OGN, OGS, OGA = 0, 2048, 5120, 5152, 6176, 6304, 6432, 6560, 6688, 6816, 6944, 6992, 8016
SCALE = 64 ** -0.5
MAGIC = 12582912.0
TWO_PI = 2.0 * math.pi


def _cw_consts():
    c1 = np.float32(6.28125)
    r = np.float64(TWO_PI) - np.float64(c1)
    c2 = np.float32(np.round(r * 2 ** 20) / 2 ** 20)
    c3 = np.float32(r - np.float64(c2))
    return float(c1), float(c2), float(c3)


class KB:
    ENG = ('pe', 'act', 'dve', 'pool', 'sp')

    def __init__(self, nc):
        self.nc = nc
        self.root = ExitStack()
        self.cnt = {}
        self.known = {e: {} for e in self.ENG}
        self.sems = {}
        self.regs = {}
        self.residue = {}
        self.eng = {'pe': nc.tensor, 'act': nc.scalar, 'dve': nc.vector, 'pool': nc.gpsimd, 'sp': nc.sync}
        self.uid = 0
        self.ninst = 0
        self.taps = {}
        self.base = {}
        self.marks = []
        self.snap = {}
        self.out_dsems = []

    def semh(self, key):
        if key not in self.sems:
            self.sems[key] = self.root.enter_context(self.nc.semaphore('s%d' % len(self.sems)))
        return self.sems[key]

    def _newreg(self, nm):
        self.regs[nm] = [{}, dict(self.residue)]

    def sb(self, name, shape, dt, stack=None):
        self.uid += 1
        nm = '%s_%d' % (name, self.uid)
        st = stack if stack is not None else self.root
        t = st.enter_context(self.nc.sbuf_tensor(nm, list(shape), dt))
        self._newreg(nm)
        self.base[nm] = name
        if stack is not None:
            stack._names.append(nm)
        return t

    def psum(self, name, shape, dt):
        t = self.root.enter_context(self.nc.psum_tensor(name, list(shape), dt))
        self._newreg(name)
        return t

    def dram(self, name, shape, dt, kind):
        t = self.nc.dram_tensor(name, list(shape), dt, kind=kind)
        self._newreg(name)
        return t.ap()

    @contextmanager
    def scope(self):
        st = ExitStack()
        st._names = []
        try:
            yield st
        finally:
            for nm in st._names:
                w, r = self.regs.pop(nm)
                for d in (w, r):
                    for k, v in d.items():
                        if self.residue.get(k, 0) < v:
                            self.residue[k] = v
            st.close()

    def op(self, eng, fn, R, W, dsem=None, S=()):
        deps = {}
        strict = 0
        if eng in ('act', 'dve', 'pool') and dsem is None:
            for ap in R:
                v = self.regs[ap.name][0].get(eng, 0)
                if v > strict:
                    strict = v
        for ap in R:
            for k, v in self.regs[ap.name][0].items():
                if deps.get(k, 0) < v:
                    deps[k] = v
        for ap in W:
            w, r = self.regs[ap.name]
            for d in (w, r):
                for k, v in d.items():
                    if deps.get(k, 0) < v:
                        deps[k] = v
        kn = self.known[eng]
        E = self.eng[eng]
        if strict and kn.get(eng, 0) < strict:
            E.wait_ge(self.semh(eng), strict)
            kn[eng] = strict
        for k, v in sorted(deps.items(), key=lambda kv: -kv[1]):
            if k == eng:
                continue
            if kn.get(k, 0) < v:
                E.wait_ge(self.semh(k), v)
                kn[k] = v
                sn = self.snap.get((k, v))
                if sn:
                    for k2, v2 in sn.items():
                        if kn.get(k2, 0) < v2:
                            kn[k2] = v2
        key = dsem or eng
        inc = 16 if dsem else 1
        val = self.cnt.get(key, 0) + inc
        self.cnt[key] = val
        fn(E).then_inc(self.semh(key), inc)
        sn = dict(kn)
        if key == eng:
            sn[eng] = val - 1
        self.snap[(key, val)] = sn
        self.ninst += 1
        for ap in R:
            self.regs[ap.name][1][key] = val
        for ap in W:
            self.regs[ap.name] = [{key: val}, {}]

    def mm(self, out, lhsT, rhs, start=True, stop=True, extraR=()):
        self.op('pe', lambda e: e.matmul(out, lhsT=lhsT, rhs=rhs, start=start, stop=stop, skip_group_check=True),
                [lhsT, rhs] + list(extraR), [out])

    def tr(self, out, in_, ident):
        self.op('pe', lambda e: e.transpose(out, in_, ident), [in_, ident], [out])

    def act(self, out, in_, func, bias=None, scale=None, accum_out=None):
        R = [in_]
        S = []
        kw = {}
        if bias is not None:
            kw['bias'] = bias
            if not isinstance(bias, (int, float)):
                R.append(bias)
                S.append(bias)
        if scale is not None:
            kw['scale'] = scale
            if not isinstance(scale, (int, float)):
                R.append(scale)
                S.append(scale)
        W = [out]
        if accum_out is not None:
            kw['accum_out'] = accum_out
            W.append(accum_out)
        self.op('act', lambda e: e.activation(out=out, in_=in_, func=func, **kw), R, W, S=S)

    def tt(self, eng, out, in0, in1, op):
        self.op(eng, lambda e: e.tensor_tensor(out=out, in0=in0, in1=in1, op=op), [in0, in1], [out])

    def ts(self, eng, out, in0, s1, op0, s2=None, op1=None):
        S = [s for s in (s1, s2) if s is not None and not isinstance(s, (int, float))]
        R = [in0] + S
        if op1 is None:
            self.op(eng, lambda e: e.tensor_scalar(out=out, in0=in0, scalar1=s1, scalar2=None, op0=op0), R, [out], S=S)
        else:
            self.op(eng, lambda e: e.tensor_scalar(out=out, in0=in0, scalar1=s1, scalar2=s2, op0=op0, op1=op1), R, [out], S=S)

    def stt(self, out, in0, scalar, in1, op0, op1):
        S = [] if isinstance(scalar, (int, float)) else [scalar]
        R = [in0, in1] + S
        self.op('dve', lambda e: e.scalar_tensor_tensor(out=out, in0=in0, scalar=scalar, in1=in1, op0=op0, op1=op1), R, [out], S=S)

    def copy(self, eng, out, in_):
        if eng == 'act':
            self.op('act', lambda e: e.copy(out=out, in_=in_), [in_], [out])
        else:
            self.op(eng, lambda e: e.tensor_copy(out=out, in_=in_), [in_], [out])

    def memset(self, eng, ap, val):
        self.op(eng, lambda e: e.memset(ap, val), [], [ap])

    def dma(self, out, in_, eng='sp', dsem=None):
        if dsem is None:
            dsem = 'd_' + self.base.get(out.name, out.name)
        self.op(eng, lambda e: e.dma_start(out=out, in_=in_), [in_], [out], dsem=dsem)
        return dsem

    def tap(self, name, ap, dt=F32):
        shp = list(ap.shape)
        d = self.dram('tap_' + name, shp, dt, "ExternalOutput")
        ds = self.dma(d, ap, dsem='d_tap_' + name)
        self.out_dsems.append(ds)
        self.taps[name] = shp

    def mark(self, label):
        self.marks.append((label, dict(self.cnt)))

    def finish(self):
        E = self.eng['sp']
        for ds in self.out_dsems:
            v = self.cnt[ds]
            if self.known['sp'].get(ds, 0) < v:
                E.wait_ge(self.semh(ds), v)
                self.known['sp'][ds] = v
        self.root.close()


def bc(ap, shape):
    return ap.to_broadcast(list(shape))


def _swap64(cols):
    c = np.asarray(cols).reshape(-1, 64)
    return np.concatenate([c[:, 32:], c[:, :32]], axis=1).reshape(-1)


def _fm_chunk_cols():
    ssd = [OXBC + i * 128 + np.arange(128) for i in range(24)]
    ssd.append(np.tile(ODT + np.arange(32), 4))
    nsa = []
    for i in range(8):
        c = OQ + i * 128 + np.arange(128)
        nsa += [c, _swap64(c)]
    for off in (OKS, OKW):
        for g in range(2):
            c = np.tile(off + g * 64 + np.arange(64), 2)
            nsa += [c, _swap64(c)]
    c = OKC + np.arange(128)
    nsa += [c, _swap64(c)]
    nsa.append(OVC + np.arange(128))
    gate = [OGS + i * 128 + np.arange(128) for i in range(8)] + [OGA + i * 128 + np.arange(128) for i in range(8)]
    return ssd, nsa, gate


N_SSD, N_NSA, N_GATE = 25, 27, 16
NFC = N_SSD + N_NSA + N_GATE


def _kmaj(w):
    K, N = w.shape
    return np.ascontiguousarray(w.reshape(K // 128, 128, N).transpose(1, 0, 2))


def _cols(v, n):
    return np.ascontiguousarray(np.asarray(v).reshape(n, 128).T)


def prep_shared(inp):
    f = np.float32
    w_in = np.asarray(inp['w_in'][0], f)
    ssd, nsa, gate = _fm_chunk_cols()
    wfm = np.stack([_kmaj(w_in[:, c]) for c in (ssd + nsa + gate)], 0)
    sh = {}
    sh['wfm'] = wfm.reshape(NFC, 128, 1024)
    sh['wtz'] = np.stack([_kmaj(w_in[:, OZ + g * 512: OZ + (g + 1) * 512]) for g in range(4)], 0).reshape(4, 128, 4096)
    tv = np.concatenate([OVS + np.arange(128), OVW + np.arange(128), OGN + np.arange(48)])
    sh['wtv'] = _kmaj(w_in[:, tv]).reshape(1, 128, 8 * 304)
    wada = np.asarray(inp['w_ada'][0], f)
    sh['wada'] = np.stack([_kmaj(wada[:, i * 128:(i + 1) * 128]) for i in range(48)], 0).reshape(48, 128, 1024)
    wso = np.asarray(inp['w_ssm_out'][0], f)
    sh['wso'] = np.stack([_kmaj(wso[:, m * 128:(m + 1) * 128]) for m in range(8)], 0).reshape(8, 128, 2048)
    wno = np.asarray(inp['w_nsa_out'][0], f)
    sh['wno'] = np.stack([_kmaj(wno[:, m * 128:(m + 1) * 128]) for m in range(8)], 0).reshape(8, 128, 1024)
    wo = np.asarray(inp['w_o'][0], f)
    sh['wo'] = np.stack([_kmaj(wo[:, m * 128:(m + 1) * 128]) for m in range(8)], 0).reshape(8, 128, 1024)
    for nm, key in (('w1k', 'cmp_w1_k'), ('w1v', 'cmp_w1_v')):
        w1 = np.asarray(inp[key][0], f).reshape(32, 64, 256).transpose(1, 0, 2)
        sh[nm] = np.ascontiguousarray(np.concatenate([w1, w1], 0)).reshape(1, 128, 32 * 256)
    w2k = np.asarray(inp['cmp_w2_k'][0], f)
    sh['w2k'] = _kmaj(np.concatenate([w2k, w2k], 1)).reshape(1, 128, 2 * 128)
    sh['w2v'] = _kmaj(np.asarray(inp['cmp_w2_v'][0], f)).reshape(1, 128, 2 * 64)
    sh['pekT'] = np.ascontiguousarray(np.asarray(inp['cmp_pe_k'][0], f).T)
    sh['pevT'] = np.ascontiguousarray(np.asarray(inp['cmp_pe_v'][0], f).T)
    sh['wg'] = np.stack([_kmaj(np.asarray(inp['w_exp_gate'][0, e], f)) for e in range(32)], 0).reshape(32, 128, 2048)
    sh['wu'] = np.stack([_kmaj(np.asarray(inp['w_exp_up'][0, e], f)) for e in range(32)], 0).reshape(32, 128, 2048)
    sh['wd'] = np.stack([_kmaj(np.asarray(inp['w_exp_down'][0, e], f)) for e in range(32)], 0).reshape(32, 128, 2048)
    wr = np.concatenate([np.asarray(inp['w_router_group'][0], f), np.asarray(inp['w_router_expert'][0], f)], 1)
    sh['wr'] = _kmaj(wr).reshape(128, 8 * 36)
    br = np.concatenate([np.asarray(inp['b_router_group'][0], f), np.asarray(inp['b_router_expert'][0], f)])
    sh['br'] = np.ascontiguousarray(np.broadcast_to(br[None, :], (128, 36)))
    vec = np.zeros((128, 160), f)
    vec[:, 0:48] = _cols(inp['b_ada'][0], 48)
    vec[:, 48:56] = _cols(inp['norm1_g'][0], 8)
    vec[:, 56:64] = _cols(inp['norm2_g'][0], 8)
    vec[:, 64:72] = _cols(inp['final_g'], 8)
    vec[:, 72:96] = _cols(inp['conv_b'][0], 24)
    vec[0:32, 96] = np.asarray(inp['dt_bias'][0], f)
    vec[0:32, 97] = np.asarray(inp['a_log'][0], f)
    sh['vec'] = vec
    cw = np.asarray(inp['conv_w'][0], f)
    sh['cw'] = np.ascontiguousarray(cw.reshape(4, 24, 128).transpose(2, 1, 0)).reshape(128, 96)
    sh['dsk'] = np.ascontiguousarray(np.broadcast_to(np.asarray(inp['d_skip'][0], f)[None, :], (128, 32)))
    sh['sng'] = np.ascontiguousarray(np.broadcast_to(np.asarray(inp['ssm_norm_g'][0], f)[None, :], (128, 2048)))
    p = np.arange(128)
    sh['ident'] = np.eye(128, dtype=f)
    tl = np.arange(512) % 128
    sh['neg4'] = np.where(tl[None, :] < p[:, None], f(-30000.0), f(0.0)).astype(f)
    mw = np.zeros((128, 8, 512), f)
    for r in range(-4, 4):
        krel = 128 * r + p[:, None]
        q = np.arange(512)[None, :]
        mw[:, r + 4, :] = ((krel <= q) & (krel > q - 512)).astype(f)
    sh['maskw'] = mw.reshape(128, 8 * 512)
    c = p - 1
    tt_ = np.arange(2048)
    sh['maskc'] = ((c[:, None] >= 0) & (16 * c[:, None] + 31 <= tt_[None, :])).astype(f)
    j = np.arange(32)
    ovl = ((16 * c[:, None] < 64 * j[None, :] + 64) & (16 * c[:, None] + 32 > 64 * j[None, :]) & (c[:, None] >= 0)).astype(f)
    sh['ovl'] = ovl
    pr = np.zeros((128, 16, 32), f)
    for qb in range(16):
        local = (128 * qb + p) // 64
        pr[:, qb, :] = np.where(j[None, :] == local[:, None], f(1e6),
                                np.where(j[None, :] > local[:, None], f(-1e30),
                                         np.where(j[None, :] == 0, f(5e5), f(0.0))))
    sh['prio'] = pr.reshape(128, 512)
    invf = (10000.0 ** (-np.arange(0, 64, 2, dtype=np.float32) / 64)).astype(f)
    rc = np.zeros((128, 2), f)
    rc[:, 0] = invf[p % 32]
    rc[:, 1] = np.where((p % 64) < 32, -1.0, 1.0)
    sh['ropec'] = rc
    return sh


SCRATCH = ['wfm', 'wtz', 'wtv', 'wso', 'wno', 'wo', 'w1k', 'w1v', 'w2k', 'w2v', 'wg', 'wu', 'wd']


def build(nseq, ntiles=None, dbg=None, stop_phase=99, dbg_tile=0, dbg_chunk=0, nsa_stop=99):
    dbg = dbg or set()
    ntiles = NT if ntiles is None else ntiles
    ntok = nseq * SEQ
    nc = bass.Bass("TRN2", target_bir_lowering=False)
    kb = KB(nc)
    C1, C2, C3 = _cw_consts()
    PI_LO = 3.1415925

    def ext_in(name, shape, dt=F32):
        return kb.dram(name, shape, dt, "ExternalInput")

    xT = ext_in('xT', [8, 128, ntok])
    cT = ext_in('cT', [128, 8 * nseq])
    pos = ext_in('pos', [nseq, SEQ], I32)
    shapes = {'wfm': [NFC, 128, 1024], 'wtz': [4, 128, 4096], 'wtv': [1, 128, 2432], 'wso': [8, 128, 2048],
              'wno': [8, 128, 1024], 'wo': [8, 128, 1024], 'w1k': [1, 128, 8192], 'w1v': [1, 128, 8192],
              'w2k': [1, 128, 256], 'w2v': [1, 128, 128], 'wg': [32, 128, 2048], 'wu': [32, 128, 2048],
              'wd': [32, 128, 2048]}
    src = {k: ext_in(k, v) for k, v in shapes.items()}
    scr = {k: kb.dram('b_' + k, v, BF16, "Internal") for k, v in shapes.items()}
    wada_d = ext_in('wada', [48, 128, 1024])
    small = {k: ext_in(k, s) for k, s in (('pekT', [64, 32]), ('pevT', [64, 32]), ('wr', [128, 288]), ('br', [128, 36]),
                                          ('vec', [128, 160]), ('cw', [128, 96]), ('dsk', [128, 32]), ('sng', [128, 2048]),
                                          ('ident', [128, 128]), ('neg4', [128, 512]), ('maskw', [128, 4096]),
                                          ('maskc', [128, 2048]), ('ovl', [128, 32]), ('prio', [128, 512]), ('ropec', [128, 2]))}
    outT = kb.dram('outT', [8, 128, ntok], F32, "ExternalOutput")

    PS = [kb.psum('ps%d' % i, [128, 512], F32) for i in range(8)]

    def psb(i):
        return PS[i][:, :].bitcast(BF16)

    identF = kb.sb('identF', [128, 128], F32)
    identB = kb.sb('identB', [128, 128], BF16)
    onesF = kb.sb('onesF', [128, 128], F32)
    NEG4 = kb.sb('neg4', [128, 512], BF16)
    maskw_d = kb.dram('b_maskw', [128, 4096], BF16, "Internal")
    maskc_d = kb.dram('b_maskc', [128, 2048], BF16, "Internal")
    ROPEC = kb.sb('ropec', [128, 2], F32)
    VEC = kb.sb('vec', [128, 160], F32)
    CW = kb.sb('cw', [128, 24, 4], F32)
    DSK = kb.sb('dsk', [128, 32], F32)
    SNG = kb.sb('sng', [128, 2048], F32)
    WR = kb.sb('wr', [128, 8, 36], F32)
    BR = kb.sb('br', [128, 36], F32)
    OVL = kb.sb('ovl', [128, 32], F32)
    W2K = kb.sb('w2k', [128, 2, 128], BF16)
    W2V = kb.sb('w2v', [128, 2, 64], BF16)
    PEB = kb.sb('peb', [128, 4], F32)
    ADA = kb.sb('ada', [128, 48, nseq], F32)
    GM1 = kb.sb('gm1', [128, 8, nseq], F32)
    GM2 = kb.sb('gm2', [128, 8, nseq], F32)
    AN = kb.sb('an', [32, 1], F32)
    EPSC = kb.sb('epsc', [128, 1], F32)
    ONEC = kb.sb('onec', [128, 1], F32)

    kb.dma(identF[:], small['ident'])
    kb.dma(ROPEC[:], small['ropec'])
    kb.dma(VEC[:], small['vec'])
    kb.dma(CW[:].rearrange("p a b -> p (a b)"), small['cw'])
    kb.dma(DSK[:], small['dsk'])
    kb.dma(SNG[:], small['sng'])
    kb.dma(WR[:].rearrange("p a b -> p (a b)"), small['wr'])
    kb.dma(BR[:], small['br'])
    kb.dma(OVL[:], small['ovl'])
    kb.copy('dve', identB[:], identF[:])
    kb.memset('dve', onesF[:], 1.0)
    kb.memset('dve', EPSC[:], EPS)
    kb.memset('dve', ONEC[:], 1.0)
    with kb.scope() as sc:
        stg = kb.sb('stgc', [128, 512], F32, sc)
        kb.dma(stg[:], small['neg4'])
        kb.copy('dve', NEG4[:], stg[:])
        for dst, nm, n in ((maskw_d, 'maskw', 4096), (maskc_d, 'maskc', 2048)):
            for c0 in range(0, n, 2048):
                stb = kb.sb('stgb_%s_%d' % (nm, c0), [128, 2048], BF16, sc)
                kb.dma(stb[:], small[nm][:, c0:c0 + 2048], eng='pool')
                kb.dma(dst[:, c0:c0 + 2048], stb[:])

    with kb.scope() as sc:
        stage = [kb.sb('stage%d' % i, [128, 8192], BF16, sc) for i in range(2)]
        si = 0
        for k in SCRATCH:
            G, _, R = shapes[k]
            gp = max(1, 8192 // R)
            for g0 in range(0, G, gp):
                g1 = min(G, g0 + gp)
                st = stage[si % 2]
                si += 1
                v = st[:, 0:(g1 - g0) * R].rearrange("p (g r) -> p g r", g=g1 - g0)
                kb.dma(v, src[k][g0:g1].rearrange("g p r -> p g r"), eng='pool')
                kb.dma(scr[k][g0:g1].rearrange("g p r -> p g r"), v, eng='sp')
    kb.dma(W2K[:].rearrange("p a b -> p (a b)"), scr['w2k'][0])
    kb.dma(W2V[:].rearrange("p a b -> p (a b)"), scr['w2v'][0])

    kb.act(AN[:], VEC[0:32, 97:98], AF.Exp)
    kb.ts('dve', AN[:], AN[:], -1.0, ALU.mult)
    with kb.scope() as sc:
        CTs = kb.sb('cts', [128, 8, nseq], F32, sc)
        kb.dma(CTs[:].rearrange("p a b -> p (a b)"), cT)
        SC_ = kb.sb('sc', [128, 8, nseq], F32, sc)
        kb.act(SC_[:], CTs[:], AF.Silu)
        wslots = [kb.sb('wadas%d' % i, [128, 8, 128], F32, sc) for i in range(3)]
        for fch in range(48):
            ws = wslots[fch % 3]
            kb.dma(ws[:].rearrange("p a b -> p (a b)"), wada_d[fch])
            bank = PS[fch % 2]
            for kc in range(8):
                kb.mm(bank[:, 0:nseq], ws[:, kc, :], SC_[:, kc, :], start=(kc == 0), stop=(kc == 7))
            kb.ts('dve', ADA[:, fch, :], bank[:, 0:nseq], VEC[:, fch:fch + 1], ALU.add)
        for b in range(nseq):
            kb.stt(GM1[:, :, b], ADA[:, 8:16, b], 1.0, VEC[:, 48:56], ALU.add, ALU.mult)
            kb.stt(GM2[:, :, b], ADA[:, 32:40, b], 1.0, VEC[:, 56:64], ALU.add, ALU.mult)
        for kv, (wn, pn) in enumerate((('w1k', 'pekT'), ('w1v', 'pevT'))):
            W1 = kb.sb('w1s%d' % kv, [128, 32, 256], BF16, sc)
            kb.dma(W1[:].rearrange("p a b -> p (a b)"), scr[wn][0])
            pef = kb.sb('pef%d' % kv, [64, 32], F32, sc)
            kb.dma(pef[:], small[pn])
            peb = kb.sb('pebf', [64, 32], BF16, sc)
            kb.copy('dve', peb[:], pef[:])
            for m in range(2):
                for l in range(32):
                    kb.mm(PS[2][:, m:m + 1], W1[0:64, l, m * 128:(m + 1) * 128], peb[:, l:l + 1], start=(l == 0), stop=(l == 31))
            kb.copy('dve', PEB[:, kv * 2:kv * 2 + 2], PS[2][:, 0:2])
    if 'ada' in dbg:
        kb.tap('ada', ADA[:].rearrange("p a b -> p (a b)"))
        kb.tap('peb', PEB[:])

    KC_T = kb.sb('kct', [128, 16 + SEQ], BF16)
    VC_T = kb.sb('vct', [128, 16 + SEQ], BF16)
    KS = [kb.sb('ks%d' % g, [128, SEQ], BF16) for g in range(2)]
    KW = [kb.sb('kw%d' % g, [128, SEQ], BF16) for g in range(2)]
    VS_AUG = kb.sb('vsaug', [128, 16, 2, 65], BF16)
    VW_AUG = kb.sb('vwaug', [128, 16, 2, 65], BF16)
    KCMP = [kb.sb('kcmp%d' % g, [128, 128], BF16) for g in range(2)]
    VCMP = kb.sb('vcmp', [128, 2, 97], BF16)
    STATE = kb.sb('state', [128, 2048], F32)
    STATE_B = kb.sb('stateb', [128, 2048], BF16)
    HALO = kb.sb('halo', [128, 24, 3], BF16)
    H1AVP = kb.sb('h1avp', [128, 4, 128], BF16)
    HN = kb.sb('hn', [128, 8, T], BF16)
    YSSM = kb.sb('yssm', [128, 8, T], BF16)
    WF = [kb.sb('wf%d' % i, [128, 4, 8, 128], BF16) for i in range(2)]
    WM = [kb.sb('wm%d' % i, [128, 16, 128], BF16) for i in range(2)]

    kb.memset('pool', VS_AUG[:].rearrange("p a b c -> p (a b c)"), 1.0)
    kb.memset('pool', VW_AUG[:].rearrange("p a b c -> p (a b c)"), 1.0)

    wf_ctr = [0]

    def load_wf(c0, n):
        s = WF[wf_ctr[0] % 2]
        wf_ctr[0] += 1
        kb.dma(s[:, 0:n].rearrange("p c k m -> p c (k m)"), scr['wfm'][c0:c0 + n].rearrange("c p r -> p c r"))
        return s

    wm_ctr = [0]

    def load_wm(name, m, nk):
        s = WM[wm_ctr[0] % 2]
        wm_ctr[0] += 1
        kb.dma(s[:, 0:nk].rearrange("p k m -> p (k m)"), scr[name][m])
        return s

    def rmsnorm_rstd(src_t, sc, bankidx=0):
        SQ = [kb.sb('sq%d' % i, [128, T], F32, sc) for i in range(2)]
        for kc in range(8):
            kb.act(SQ[kc % 2][:], src_t[:, kc, :], AF.Square)
            kb.mm(PS[bankidx][:, :], onesF[:, :], SQ[kc % 2][:], start=(kc == 0), stop=(kc == 7))
        RT = kb.sb('rt', [128, T], F32, sc)
        kb.act(RT[:], PS[bankidx][:, :], AF.Sqrt, bias=EPSC[:, 0:1], scale=1.0 / D)
        RSTD = kb.sb('rstd', [128, T], F32, sc)
        kb.op('dve', lambda e: e.reciprocal(out=RSTD[:], in_=RT[:]), [RT[:]], [RSTD[:]])
        return RSTD

    def seq_init(b):
        kb.memset('pool', KC_T[:, 0:16], 0.0)
        kb.memset('pool', VC_T[:, 0:16], 0.0)
        for g in range(2):
            kb.memset('pool', KCMP[g][:], 0.0)
        kb.memset('pool', VCMP[:].rearrange("p a b -> p (a b)"), 0.0)
        for g in range(2):
            kb.memset('pool', VCMP[:, g, 64:65], 1.0)
            kb.copy('pool', VCMP[:, g, 65:97], OVL[:])
        kb.memset('pool', STATE[:], 0.0)
        kb.memset('pool', STATE_B[:], 0.0)
        kb.memset('pool', HALO[:].rearrange("p a b -> p (a b)"), 0.0)

    def phase1_norm1(b, tt, tok0):
        with kb.scope() as sc:
            XT = kb.sb('xt', [128, 8, T], F32, sc)
            kb.dma(XT[:], xT[:, :, tok0:tok0 + T].rearrange("k p t -> p k t"))
            RSTD = rmsnorm_rstd(XT, sc)
            TMP = [kb.sb('n1tmp%d' % i, [128, T], F32, sc) for i in range(2)]
            for kc in range(8):
                tm = TMP[kc % 2]
                kb.tt('dve', tm[:], XT[:, kc, :], RSTD[:], ALU.mult)
                kb.act(HN[:, kc, :], tm[:], AF.Identity, bias=ADA[:, 0 + kc, b:b + 1], scale=GM1[:, kc, b:b + 1])
        if 'hn' in dbg and b == 0 and tt == dbg_tile:
            kb.tap('hn', HN[:].rearrange("p a b -> p (a b)"), BF16)

    def phase2_ssd_proj(b, tt, sc):
        XBC = kb.sb('xbc', [128, 24, T], BF16, sc)
        ZS = kb.sb('zs', [128, 4, 2048], BF16, sc)
        DT_T = kb.sb('dtT', [128, T], F32, sc)
        kb.memset('pool', DT_T[:, :], 0.0)
        DTA_T = kb.sb('dtaT', [32, T], F32, sc)
        with kb.scope() as s2:
            U = [kb.sb('u%d' % i, [128, T + 3], BF16, s2) for i in range(4)]
            DG = [kb.sb('dg%d' % i, [128, 4, 128], BF16, s2) for i in range(4)]
            for i in range(N_SSD):
                if i % 4 == 0:
                    wf = load_wf(i, min(4, N_SSD - i))
                bankA = PS[i % 4]
                for kc in range(8):
                    kb.mm(bankA[:, :], wf[:, i % 4, kc, :], HN[:, kc, :], start=(kc == 0), stop=(kc == 7))
                if i < 24:
                    u = U[i % 4]
                    dg = DG[i % 4]
                    kb.copy('act', u[:, 3:T + 3], bankA[:, :])
                    kb.copy('dve', u[:, 0:3], HALO[:, i, :])
                    kb.copy('dve', HALO[:, i, :], u[:, T:T + 3])
                    for k in range(4):
                        kb.ts('dve', dg[:, k, :], identB[:, :], CW[:, i, k:k + 1], ALU.mult)
                ip = i - 1
                if 0 <= ip < 24:
                    up, dgp = U[ip % 4], DG[ip % 4]
                    bankB = PS[4 + ip % 2]
                    for k in range(4):
                        kb.mm(bankB[:, :], dgp[:, k, :], up[:, k:k + T], start=(k == 0), stop=(k == 3))
                    kb.act(XBC[:, ip, :], bankB[:, :], AF.Silu, bias=VEC[:, 72 + ip:73 + ip])
                if i >= 24:
                    XD = kb.sb('xd', [32, T], F32, s2)
                    MX = kb.sb('mx', [32, T], F32, s2)
                    NA = kb.sb('na', [32, T], F32, s2)
                    kb.ts('dve', XD[:], bankA[0:32, :], VEC[0:32, 96:97], ALU.add)
                    kb.ts('dve', MX[:], XD[:], 0.0, ALU.max)
                    kb.stt(NA[:], MX[:], -2.0, XD[:], ALU.mult, ALU.add)
                    kb.act(NA[:], NA[:], AF.Exp)
                    kb.act(NA[:], NA[:], AF.Ln, bias=ONEC[0:32, 0:1])
                    kb.tt('dve', DT_T[0:32, :], MX[:], NA[:], ALU.add)
                    kb.ts('dve', DTA_T[:], DT_T[0:32, :], AN[:, 0:1], ALU.mult)
            WTZ = [kb.sb('wtz%d' % i, [128, 8, 512], BF16, s2) for i in range(2)]
            for g in range(4):
                wz = WTZ[g % 2]
                kb.dma(wz[:].rearrange("p k n -> p (k n)"), scr['wtz'][g])
                for c in range(4):
                    bank = PS[6 + c % 2]
                    for kc in range(8):
                        kb.mm(bank[:, :], HN[:, kc, c * 128:(c + 1) * 128], wz[:, kc, :], start=(kc == 0), stop=(kc == 7))
                    kb.act(ZS[:, c, g * 512:(g + 1) * 512], bank[:, :], AF.Silu)
        if b == 0 and tt == dbg_tile:
            if 'xbc' in dbg:
                kb.tap('xbc', XBC[:].rearrange("p a b -> p (a b)"), BF16)
            if 'dt' in dbg:
                kb.tap('dt', DT_T[0:32, :])
            if 'zs' in dbg:
                kb.tap('zs', ZS[:].rearrange("p a b -> p (a b)"), BF16)
        return XBC, ZS, DT_T, DTA_T

    def phase3_ssd(b, tt, sc, XBC, ZS, DT_T, DTA_T):
        YNT = kb.sb('ynt', [128, 16, T], BF16, sc)
        with kb.scope() as s3:
            XS = kb.sb('xs', [128, 2048], BF16, s3)
            XDT = kb.sb('xdt', [128, 2048], BF16, s3)
            XW = kb.sb('xw', [128, 2048], BF16, s3)
            BTOK = kb.sb('btok', [128, 4, 128], BF16, s3)
            ACUMT = kb.sb('acumT', [128, T], F32, s3)
            kb.memset('pool', ACUMT[:, :], 0.0)
            ATOK = kb.sb('atok', [128, 32], F32, s3)
            DTOK = kb.sb('dtok', [128, 32], F32, s3)
            NEGA = kb.sb('nega', [128, 32], F32, s3)
            EA = kb.sb('ea', [128, 32], F32, s3)
            D1 = kb.sb('d1', [128, 32], F32, s3)
            WEND = kb.sb('wend', [128, 32], F32, s3)
            DECS = kb.sb('decs', [128, 32], F32, s3)
            DIAGA = kb.sb('diaga', [128, 32], F32, s3)
            kb.memset('pool', DIAGA[:, :], 0.0)
            CBT = kb.sb('cbt', [128, 4, 128], F32, s3)
            LT = [kb.sb('lt%d' % i, [128, 4, 128], BF16, s3) for i in range(3)]
            MT = [kb.sb('mt%d' % i, [128, 4, 128], BF16, s3) for i in range(3)]
            YT = kb.sb('yt', [128, 2048], F32, s3)
            TMPA = [kb.sb('s3tmp%d' % i, [128, 512], F32, s3) for i in range(2)]
            YN = XDT
            SS = kb.sb('ss', [128, 4], F32, s3)
            RS = kb.sb('rs', [128, 4], F32, s3)
            qc = [0]
            for c in range(4):
                cs = slice(c * 128, (c + 1) * 128)
                for i in range(16):
                    kb.tr(psb(i // 8)[:, (i % 8) * 128:(i % 8 + 1) * 128], XBC[:, i, cs], identB[:, :])
                kb.copy('act', XS[:, 0:1024], psb(0)[:, :])
                kb.copy('dve', XS[:, 1024:2048], psb(1)[:, :])
                for g in range(4):
                    kb.tr(psb(0)[:, g * 128:(g + 1) * 128], XBC[:, 16 + g, cs], identB[:, :])
                kb.copy('act', BTOK[:].rearrange("p a b -> p (a b)"), psb(0)[:, 0:512])
                kb.op('dve', lambda e: e.tensor_tensor_scan(out=ACUMT[0:32, cs], data0=onesF[0:32, 0:128], data1=DTA_T[:, cs],
                                                            initial=0.0, op0=ALU.mult, op1=ALU.add),
                      [onesF[:, :], DTA_T[:, :]], [ACUMT[:, :]])
                kb.tr(PS[1][:, 0:128], ACUMT[:, cs], identF[:, :])
                kb.tr(PS[1][:, 128:256], DT_T[:, cs], identF[:, :])
                kb.copy('dve', ATOK[:], PS[1][:, 0:32])
                kb.copy('dve', DTOK[:], PS[1][:, 128:160])
                kb.ts('dve', NEGA[:], ATOK[:], -1.0, ALU.mult)
                kb.act(EA[:], ATOK[:], AF.Exp)
                kb.ts('dve', DIAGA[0:32, :], identF[0:32, 0:32], ACUMT[0:32, c * 128 + 127:c * 128 + 128], ALU.mult)
                kb.mm(PS[1][:, 256:288], onesF[:, 0:128], DIAGA[:, :])
                kb.tt('dve', D1[:], PS[1][:, 256:288], ATOK[:], ALU.subtract)
                kb.act(D1[:], D1[:], AF.Exp)
                kb.tt('dve', WEND[:], D1[:], DTOK[:], ALU.mult)
                kb.act(DECS[:], PS[1][:, 256:288], AF.Exp)
                kb.tt('dve', XDT[:].rearrange("p (h d) -> p h d", h=32), XS[:].rearrange("p (h d) -> p h d", h=32),
                      bc(DTOK[:].unsqueeze(2), [128, 32, 64]), ALU.mult)
                kb.tt('pool', XW[:].rearrange("p (h d) -> p h d", h=32), XS[:].rearrange("p (h d) -> p h d", h=32),
                      bc(WEND[:].unsqueeze(2), [128, 32, 64]), ALU.mult)
                for g in range(4):
                    kb.mm(PS[6][:, g * 128:(g + 1) * 128], XBC[:, 16 + g, cs], XBC[:, 20 + g, cs])
                kb.copy('act', CBT[:].rearrange("p a b -> p (a b)"), PS[6][:, :])
                SEGB = [PS[2], PS[3], PS[6]]

                def quad_unit(q):
                    g, q2 = q // 2, q % 2
                    gs_ = slice(g * 512, (g + 1) * 512)
                    st = {}

                    def s1():
                        sl = qc[0] % 3
                        qc[0] += 1
                        lt, mt, bankS = LT[sl], MT[sl], SEGB[sl]
                        st['mt'] = mt
                        for j in range(4):
                            kb.mm(bankS[:, j * 128:(j + 1) * 128], bc(identF[:, 4 * q + j:4 * q + j + 1], [128, 128]), ACUMT[:, cs],
                                  start=(j == 0), stop=False)
                        kb.mm(bankS[:, :], identB[:, :], NEG4[:, :], start=False, stop=True)
                        for j in range(4):
                            h = 4 * q + j
                            kb.act(lt[:, j, :], bankS[:, j * 128:(j + 1) * 128], AF.Exp, bias=NEGA[:, h:h + 1])
                        kb.tt('dve', mt[:], lt[:], bc(CBT[:, g, :].unsqueeze(1), [128, 4, 128]), ALU.mult)

                    def s2():
                        mt = st['mt']
                        bankY = PS[4 + g % 2]
                        for j in range(4):
                            h = 4 * q + j
                            kb.mm(bankY[:, (4 * q2 + j) * 64:(4 * q2 + j + 1) * 64], mt[:, j, :], XDT[:, h * 64:(h + 1) * 64])
                        if q2 == 1:
                            kb.mm(PS[7][:, :], XBC[:, 20 + g, cs], STATE_B[:, gs_])
                            tm = TMPA[g % 2]
                            kb.tt('dve', tm[:].rearrange("p (h d) -> p h d", h=8), PS[7][:, :].rearrange("p (h d) -> p h d", h=8),
                                  bc(EA[:, 8 * g:8 * g + 8].unsqueeze(2), [128, 8, 64]), ALU.mult)
                            kb.tt('dve', YT[:, gs_], bankY[:, :], tm[:], ALU.add)
                            kb.mm(PS[0][:, :], BTOK[:, g, :], XW[:, gs_])
                            tm2 = TMPA[(g + 1) % 2]
                            kb.tt('pool', tm2[:].rearrange("p (h d) -> p h d", h=8), STATE[:, gs_].rearrange("p (h d) -> p h d", h=8),
                                  bc(DECS[:, 8 * g:8 * g + 8].unsqueeze(2), [128, 8, 64]), ALU.mult)
                            kb.tt('dve', STATE[:, gs_], tm2[:], PS[0][:, :], ALU.add)
                            kb.copy('pool', STATE_B[:, gs_], STATE[:, gs_])
                    return (s1, s2)

                units = [quad_unit(q) for q in range(8)]
                for i in range(2):
                    units[i][0]()
                for i in range(8):
                    units[i][1]()
                    if i + 2 < 8:
                        units[i + 2][0]()
                for g in range(4):
                    gs_ = slice(g * 512, (g + 1) * 512)
                    tm = TMPA[g % 2]
                    kb.tt('dve', tm[:].rearrange("p (h d) -> p h d", h=8), XS[:, gs_].rearrange("p (h d) -> p h d", h=8),
                          bc(DSK[:, 8 * g:8 * g + 8].unsqueeze(2), [128, 8, 64]), ALU.mult)
                    kb.tt('dve', YT[:, gs_], YT[:, gs_], tm[:], ALU.add)
                kb.tt('dve', YT[:], YT[:], ZS[:, c, :], ALU.mult)
                kb.act(XW[:], YT[:], AF.Square)
                kb.op('dve', lambda e: e.tensor_reduce(out=SS[:], in_=XW[:].rearrange("p (g d) -> p g d", g=4), axis=AX.X, op=ALU.add),
                      [XW[:]], [SS[:]])
                kb.act(RS[:], SS[:], AF.Sqrt, bias=EPSC[:, 0:1], scale=1.0 / 512)
                kb.op('dve', lambda e: e.reciprocal(out=RS[:], in_=RS[:]), [RS[:]], [RS[:]])
                for g in range(4):
                    gs_ = slice(g * 512, (g + 1) * 512)
                    kb.stt(YN[:, gs_], YT[:, gs_], RS[:, g:g + 1], SNG[:, gs_], ALU.mult, ALU.mult)
                if b == 0 and tt == dbg_tile and c == dbg_chunk:
                    if 'yt' in dbg:
                        kb.tap('yt', YT[:])
                    if 'yn' in dbg:
                        kb.tap('yn', YN[:], BF16)
                    if 'atok' in dbg:
                        kb.tap('atok', ATOK[:])
                for i in range(16):
                    kb.tr(psb(i // 8)[:, (i % 8) * 128:(i % 8 + 1) * 128], YN[:, i * 128:(i + 1) * 128], identB[:, :])
                kb.copy('act', YNT[:, 0:8, cs], psb(0)[:, :].rearrange("p (a b) -> p a b", a=8))
                kb.copy('act', YNT[:, 8:16, cs], psb(1)[:, :].rearrange("p (a b) -> p a b", a=8))
        for m in range(8):
            w = load_wm('wso', m, 16)
            bank = PS[m % 4]
            for kc in range(16):
                kb.mm(bank[:, :], w[:, kc, :], YNT[:, kc, :], start=(kc == 0), stop=(kc == 15))
            kb.copy('act', YSSM[:, m, :], bank[:, :])
        if 'yssm' in dbg and b == 0 and tt == dbg_tile:
            kb.tap('yssm', YSSM[:].rearrange("p a b -> p (a b)"), BF16)

    def rope_tables(b, tt, st):
        t0 = tt * T
        COS = kb.sb('cos', [128, T], F32, st)
        SINS = kb.sb('sins', [128, T], F32, st)
        with kb.scope() as sr:
            POSI = kb.sb('posi', [128, T], I32, sr)
            kb.dma(POSI[:], pos[b:b + 1, t0:t0 + T].partition_broadcast(128))
            ANG = kb.sb('ang', [128, T], F32, sr)
            TK = kb.sb('tk', [128, T], F32, sr)
            RR = kb.sb('rr', [128, T], F32, sr)
            kb.copy('dve', ANG[:], POSI[:])
            kb.ts('dve', ANG[:], ANG[:], ROPEC[:, 0:1], ALU.mult)
            for which in (0, 1):
                if which == 0:
                    kb.ts('dve', TK[:], ANG[:], 1.0 / TWO_PI, ALU.mult, MAGIC, ALU.add)
                else:
                    kb.ts('dve', TK[:], ANG[:], 1.0 / TWO_PI, ALU.mult, 0.25, ALU.add)
                    kb.ts('dve', TK[:], TK[:], MAGIC, ALU.add)
                kb.ts('dve', TK[:], TK[:], -MAGIC, ALU.add)
                kb.stt(RR[:], TK[:], -C1, ANG[:], ALU.mult, ALU.add)
                kb.stt(RR[:], TK[:], -C2, RR[:], ALU.mult, ALU.add)
                kb.stt(RR[:], TK[:], -C3, RR[:], ALU.mult, ALU.add)
                if which == 1:
                    kb.ts('dve', RR[:], RR[:], math.pi / 2, ALU.add)
                kb.ts('dve', RR[:], RR[:], PI_LO, ALU.min, -PI_LO, ALU.max)
                if which == 0:
                    kb.act(SINS[:], RR[:], AF.Sin, scale=ROPEC[:, 1:2])
                else:
                    kb.act(COS[:], RR[:], AF.Sin)
        return COS, SINS

    def phase4_nsa(b, tt, sc, tok0, COS, SINS):
        t0 = tt * T
        ATTT = kb.sb('attT', [128, 8, T], BF16, sc)
        with kb.scope() as s4:
            QX = kb.sb('qx', [128, 2, 8, T], BF16, s4)
            kb.memset('pool', QX[64:128, 0].rearrange("p a b -> p (a b)"), 0.0)
            kb.memset('pool', QX[0:64, 1].rearrange("p a b -> p (a b)"), 0.0)
            GATE = kb.sb('gate', [128, 4, 48], F32, s4)
            if b == 0 and tt == dbg_tile and 'cos' in dbg:
                kb.tap('cos', COS[:])
                kb.tap('sins', SINS[:])
            if nsa_stop <= 1:
                return ATTT
            sw_scope = kb.scope()
            sw = sw_scope.__enter__()
            W1S = []
            for kv, wn in enumerate(('w1k', 'w1v')):
                W1_ = kb.sb('w1_%d' % kv, [128, 32, 256], BF16, sw)
                kb.dma(W1_[:].rearrange("p a b -> p (a b)"), scr[wn][0])
                W1S.append(W1_)
            with kb.scope() as sp_:
                T1 = [kb.sb('rt1_%d' % i, [128, T], F32, sp_) for i in range(2)]
                T2 = [kb.sb('rt2_%d' % i, [128, T], F32, sp_) for i in range(2)]
                dests = [None for i in range(8)] + [KS[0][:, t0:t0 + T], KS[1][:, t0:t0 + T], KW[0][:, t0:t0 + T],
                                                          KW[1][:, t0:t0 + T], KC_T[:, 16 + t0:16 + t0 + T]]
                for pi in range(14):
                    ci = 2 * pi
                    for cc in (ci, ci + 1):
                        if cc < N_NSA and cc % 4 == 0:
                            wf = load_wf(N_SSD + cc, min(4, N_NSA - cc))
                        if cc < N_NSA:
                            bank = PS[(cc % 4)]
                            for kc in range(8):
                                kb.mm(bank[:, :], wf[:, cc % 4, kc, :], HN[:, kc, :], start=(kc == 0), stop=(kc == 7))
                    if pi < 13:
                        bA, bB = PS[ci % 4], PS[(ci + 1) % 4]
                        t1, t2 = T1[pi % 2], T2[pi % 2]
                        kb.tt('dve', t1[:], bA[:, :], COS[:], ALU.mult)
                        kb.tt('dve', t2[:], bB[:, :], SINS[:], ALU.mult)
                        if pi < 8:
                            kb.tt('pool', QX[0:64, 0, pi, :], t1[0:64, :], t2[0:64, :], ALU.add)
                            kb.tt('pool', QX[64:128, 1, pi, :], t1[64:128, :], t2[64:128, :], ALU.add)
                        else:
                            kb.tt('pool', dests[pi], t1[:], t2[:], ALU.add)
                    else:
                        kb.copy('act', VC_T[:, 16 + t0:16 + t0 + T], PS[ci % 4][:, :])
                WTV = kb.sb('wtv', [128, 8, 304], BF16, sp_)
                kb.dma(WTV[:].rearrange("p k n -> p (k n)"), scr['wtv'][0])
                for c in range(4):
                    bank = PS[4 + c % 2]
                    kbk = 4 * tt + c
                    for kc in range(8):
                        kb.mm(bank[:, 0:304], HN[:, kc, c * 128:(c + 1) * 128], WTV[:, kc, :], start=(kc == 0), stop=(kc == 7))
                    kb.copy('act', VS_AUG[:, kbk, :, 0:64], bank[:, 0:128].rearrange("p (g d) -> p g d", g=2))
                    kb.copy('act', VW_AUG[:, kbk, :, 0:64], bank[:, 128:256].rearrange("p (g d) -> p g d", g=2))
                    kb.act(GATE[:, c, :], bank[:, 256:304], AF.Sigmoid)
            if b == 0 and tt == dbg_tile and 'q' in dbg:
                kb.tap('kw0', KW[0][:, t0:t0 + T], BF16)
                kb.tap('gate', GATE[:].rearrange("p a b -> p (a b)"))
            if nsa_stop <= 2:
                return ATTT
            with kb.scope() as scp:
                H1A = kb.sb('h1a', [128, 4, 32], BF16, scp)
                for kv, (wn, SRC) in enumerate((('w1k', KC_T), ('w1v', VC_T))):
                    W1 = W1S[kv]
                    for g in range(2):
                        for m in range(2):
                            col = (g * 2 + m) * 32
                            for l in range(32):
                                kb.mm(PS[6 - g][:, col:col + 32], W1[64 * g:64 * g + 64, l, m * 128:(m + 1) * 128],
                                      SRC[64 * g:64 * g + 64, t0 + l:t0 + l + 497:16], start=(l == 0), stop=(l == 31))
                    for g in range(2):
                        for m in range(2):
                            col = (g * 2 + m) * 32
                            dst = H1A[:, g * 2 + m, :] if kv == 0 else H1AVP[:, g * 2 + m, 32 * tt:32 * tt + 32]
                            kb.act(dst, PS[6 - g][:, col:col + 32], AF.Silu, bias=PEB[:, kv * 2 + m:kv * 2 + m + 1])
                    if kv == 0:
                        for g in range(2):
                            for m in range(2):
                                kb.mm(PS[7][:, g * 32:(g + 1) * 32], W2K[:, m, :], H1A[:, g * 2 + m, :], start=(m == 0), stop=(m == 1))
                        for g in range(2):
                            kb.copy('act', KCMP[g][:, 32 * tt:32 * tt + 32], PS[7][:, g * 32:(g + 1) * 32])
                    else:
                        p0 = 32 * tt if tt < 3 else 64
                        p1 = 32 * tt + 32
                        for g in range(2):
                            for m in range(2):
                                kb.mm(PS[7][p0:p1, 64 + g * 64:128 + g * 64], H1AVP[:, g * 2 + m, p0:p1], W2V[:, m, :],
                                      start=(m == 0), stop=(m == 1))
                        kb.copy('act', VCMP[p0:p1, :, 0:64], PS[7][p0:p1, 64:192].rearrange("p (g d) -> p g d", g=2))
            sw_scope.__exit__(None, None, None)
            if b == 0 and tt == dbg_tile and 'kcmp' in dbg:
                kb.tap('kcmp', KCMP[0][:], BF16)
                kb.tap('vcmp', VCMP[:].rearrange("p a b -> p (a b)"), BF16)
            if nsa_stop <= 3:
                return ATTT
            MASKW = kb.sb('maskw', [128, 8, 512], BF16, s4)
            kb.dma(MASKW[:].rearrange("p a b -> p (a b)"), maskw_d)
            MASKC = kb.sb('maskc', [128, T], BF16, s4)
            kb.dma(MASKC[:], maskc_d[:, t0:t0 + T])
            PRIO = kb.sb('prio', [128, 4, 32], F32, s4)
            kb.dma(PRIO[:].rearrange("p a b -> p (a b)"), small['prio'][:, 4 * tt * 32:(4 * tt + 4) * 32])
            ATT = kb.sb('att', [128, 4, 1024], BF16, s4)
            ATTB = kb.sb('attb', [128, 4, 1024], BF16, s4)
            IMP = [kb.sb('imp%d' % g, [128, 4, 32], F32, s4) for g in range(2)]
            PT = [kb.sb('pt%d' % i, [128, T], BF16, s4) for i in range(5)]
            NRM = kb.sb('nrm', [128, 4, 97], F32, s4)
            RD = kb.sb('rd', [128, 4], F32, s4)
            CF = kb.sb('cf', [128, 4], F32, s4)
            TMO = kb.sb('tmo', [128, 4, 64], F32, s4)
            ptc = [0]

            def oview(bank, w):
                return bank[:, :].rearrange("p (q c) -> p q c", q=4)[:, :, 0:w]

            OB = [PS[4], PS[5], PS[7]]
            EPI = [(kb.sb('erd%d' % i, [128, 4], F32, s4), kb.sb('ecf%d' % i, [128, 4], F32, s4), kb.sb('etmo%d' % i, [128, 4, 64], F32, s4))
                   for i in range(4)]
            epc = [0]
            pending = []

            def drain_one():
                if pending:
                    pending.pop(0)[1]()

            def drain_bank(bank):
                while any(p[0] is bank for p in pending):
                    drain_one()
            SB5 = [PS[0], PS[1], PS[2], PS[3], PS[6]]
            hctr = [0]

            def run_units(units, D=3):
                n = len(units)
                for i in range(min(D, n)):
                    units[i][0]()
                for i in range(n):
                    units[i][1]()
                    if i + D < n:
                        units[i + D][0]()

            def cmp_unit(hc, e2):
                g = hc // 4
                h = 2 * hc + e2
                ps_ = slice(64 * e2, 64 * e2 + 64)
                st = {}

                def s1():
                    st['bankS'] = SB5[ptc[0] % 5]
                    st['pt'] = PT[ptc[0] % 5]
                    ptc[0] += 1
                    kb.mm(st['bankS'][:, :], KCMP[g][:, :], QX[:, e2, hc, :])
                    kb.act(st['pt'][:], st['bankS'][:, :], AF.Exp, scale=SCALE)
                    kb.tt('dve', st['pt'][:], st['pt'][:], MASKC[:, :], ALU.mult)

                def s2():
                    pt = st['pt']
                    bankO = OB[hctr[0] % 3]
                    hctr[0] += 1
                    for qb in range(4):
                        kb.mm(bankO[:, qb * 128:qb * 128 + 97], pt[:, qb * 128:(qb + 1) * 128], VCMP[:, g, :], start=(qb == 0), stop=(qb == 3))
                    ov = oview(bankO, 97)
                    kb.ts('dve', RD[:], ov[:, :, 64], 1e-30, ALU.max)
                    kb.op('dve', lambda e: e.reciprocal(out=RD[:], in_=RD[:]), [RD[:]], [RD[:]])
                    kb.tt('dve', NRM[:], ov, bc(RD[:].unsqueeze(2), [128, 4, 97]), ALU.mult)
                    if h % 8 == 0:
                        kb.copy('pool', IMP[g][:], NRM[:, :, 65:97])
                    else:
                        kb.tt('pool', IMP[g][:], IMP[g][:], NRM[:, :, 65:97], ALU.add)
                    kb.tt('dve', ATT[:, :, h * 64:(h + 1) * 64], NRM[:, :, 0:64], bc(GATE[:, :, 3 * h:3 * h + 1], [128, 4, 64]), ALU.mult)
                return (s1, s2)

            run_units([cmp_unit(hc, e2) for hc in range(8) for e2 in range(2)])
            if b == 0 and tt == dbg_tile and 'ocmp' in dbg:
                kb.tap('ocmp', ATT[:].rearrange("p a b -> p (a b)"), BF16)
                kb.tap('imp', IMP[0][:].rearrange("p a b -> p (a b)"))
            if nsa_stop <= 4:
                return ATTT
            nkb = 4 * tt + 4
            SELT = [kb.sb('selT%d' % g, [32, T], BF16, s4) for g in range(2)]
            MEXP = kb.sb('mexp', [128, nkb, 2, T], BF16, s4)
            IMPP = kb.sb('impp', [128, 32], F32, s4)
            MAX8 = kb.sb('max8', [128, 8], F32, s4)
            SEL = kb.sb('sel', [128, 32], BF16, s4)
            for g in range(2):
                for qb in range(4):
                    kb.tt('dve', IMPP[:], IMP[g][:, qb, :], PRIO[:, qb, :], ALU.add)
                    kb.op('dve', lambda e: e.max(out=MAX8[:], in_=IMPP[:]), [IMPP[:]], [MAX8[:]])
                    kb.ts('dve', SEL[:], IMPP[:], MAX8[:, 3:4], ALU.is_ge)
                    kb.tr(psb(6)[0:32, qb * 128:(qb + 1) * 128], SEL[:], identB[:, :])
                kb.copy('act', SELT[g][:], psb(6)[0:32, 0:512])
            if b == 0 and tt == dbg_tile and 'selt' in dbg:
                kb.tap('selt', SELT[0][:], BF16)
            for kbk in range(nkb):
                for g in range(2):
                    for hf in range(2):
                        kb.mm(PS[7][64 * hf:64 * hf + 64, :], bc(identB[0:32, 2 * kbk + hf:2 * kbk + hf + 1], [32, 64]), SELT[g][:, :])
                    kb.copy('act', MEXP[:, kbk, g, :], PS[7][:, :])
                    if kbk >= 4 * tt:
                        kb.tt('pool', MEXP[:, kbk, g, :], MEXP[:, kbk, g, :], MASKW[:, 4 + kbk - 4 * tt, :], ALU.mult)

            if nsa_stop <= 5:
                return ATTT
            def branch_units(KK, VAUG, gate_idx, kb_lo, use_mexp, last):
                units = []
                for hc in range(8):
                    g = hc // 4
                    per = []
                    for e2 in range(2):
                        h = 2 * hc + e2
                        ps_ = slice(64 * e2, 64 * e2 + 64)
                        hstate = {}
                        kbs = list(range(kb_lo, nkb))
                        per.append([br_unit(KK, VAUG, gate_idx, use_mexp, last, hc, g, h, ps_, kbk, ki == 0, ki == len(kbs) - 1, hstate)
                                    for ki, kbk in enumerate(kbs)])
                    for ua, ub in zip(per[0], per[1]):
                        units += [ua, ub]
                return units

            def br_unit(KK, VAUG, gate_idx, use_mexp, last, hc, g, h, ps_, kbk, is_first, is_last, hstate):
                e2x = h % 2
                r = kbk - 4 * tt
                q0 = max(0, r)
                q1 = 3 if use_mexp else min(3, r + 4)
                cols = slice(q0 * 128, (q1 + 1) * 128)
                st = {}

                def s1():
                    st['bankS'] = SB5[ptc[0] % 5]
                    st['pt'] = PT[ptc[0] % 5]
                    ptc[0] += 1
                    bankS, pt = st['bankS'], st['pt']
                    kb.mm(bankS[:, cols], KK[g][:, kbk * 128:(kbk + 1) * 128], QX[:, e2x, hc, cols])
                    kb.act(pt[:, cols], bankS[:, cols], AF.Exp, scale=SCALE)
                    if use_mexp:
                        kb.tt('dve', pt[:, cols], pt[:, cols], MEXP[:, kbk, g, cols], ALU.mult)
                    else:
                        mq = q0 if r >= 0 else q1
                        mc = slice(mq * 128, (mq + 1) * 128)
                        kb.tt('dve', pt[:, mc], pt[:, mc], MASKW[:, r + 4, mc], ALU.mult)

                def s2():
                    pt = st['pt']
                    if is_first:
                        hstate['bankO'] = OB[hctr[0] % 3]
                        hctr[0] += 1
                        drain_bank(hstate['bankO'])
                    bankO = hstate['bankO']
                    for qi, qb in enumerate(range(q0, q1 + 1)):
                        kb.mm(bankO[:, qb * 128:qb * 128 + 65], pt[:, qb * 128:(qb + 1) * 128], VAUG[:, kbk, g, :],
                              start=(is_first and qi == 0), stop=False)
                    else:
                        drain_one()
                    if is_last:
                        ov = oview(bankO, 65)
                        rd_, cf_, tmo_ = EPI[epc[0] % 4]
                        epc[0] += 1
                        kb.ts('dve', rd_[:], ov[:, :, 64], 1e-30, ALU.max)
                        pending.append((bankO, lambda: kb.op('dve', lambda e: e.reciprocal(out=rd_[:], in_=rd_[:]), [rd_[:]], [rd_[:]])))
                        pending.append((bankO, lambda: kb.tt('dve', cf_[:], rd_[:], GATE[:, :, 3 * h + gate_idx], ALU.mult)))

                        def fin():
                            kb.tt('dve', tmo_[:], ov[:, :, 0:64], bc(cf_[:].unsqueeze(2), [128, 4, 64]), ALU.mult)
                            dst = ATTB if last else ATT
                            kb.tt('pool', dst[:, :, h * 64:(h + 1) * 64], ATT[:, :, h * 64:(h + 1) * 64], tmo_[:], ALU.add)
                        pending.append((bankO, fin))
                return (s1, s2)

            run_units(branch_units(KW, VW_AUG, 2, max(0, 4 * tt - 4), False, False)
                      + branch_units(KS, VS_AUG, 1, 0, True, True), D=4)
            while pending:
                drain_one()
            if b == 0 and tt == dbg_tile and 'attb' in dbg:
                kb.tap('attb', ATTB[:].rearrange("p a b -> p (a b)"), BF16)
            for qb in range(4):
                for i in range(8):
                    kb.tr(psb(6)[:, i * 128:(i + 1) * 128], ATTB[:, qb, i * 128:(i + 1) * 128], identB[:, :])
                kb.copy('act', ATTT[:, :, qb * 128:(qb + 1) * 128], psb(6)[:, :].rearrange("p (a b) -> p a b", a=8))
        return ATTT

    def phase5_merge(b, tt, sc, tok0, ATTT):
        H = kb.sb('h', [128, 8, T], F32, sc)
        kb.dma(H[:], xT[:, :, tok0:tok0 + T].rearrange("k p t -> p k t"))
        with kb.scope() as s5:
            SG = kb.sb('sg', [128, 16, T], BF16, s5)
            MRG = kb.sb('mrg', [128, 8, T], BF16, s5)
            for i in range(N_GATE):
                if i % 4 == 0:
                    wf = load_wf(N_SSD + N_NSA + i, 4)
                bank = PS[i % 4]
                for kc in range(8):
                    kb.mm(bank[:, :], wf[:, i % 4, kc, :], HN[:, kc, :], start=(kc == 0), stop=(kc == 7))
                kb.act(SG[:, i, :], bank[:, :], AF.Sigmoid)
            T1 = [kb.sb('m1_%d' % i, [128, T], F32, s5) for i in range(2)]
            T2 = [kb.sb('m2_%d' % i, [128, T], F32, s5) for i in range(2)]
            for m in range(8):
                w = load_wm('wno', m, 8)
                bank = PS[4 + m % 4]
                for kc in range(8):
                    kb.mm(bank[:, :], w[:, kc, :], ATTT[:, kc, :], start=(kc == 0), stop=(kc == 7))
                t1, t2 = T1[m % 2], T2[m % 2]
                kb.tt('dve', t1[:], bank[:, :], SG[:, 8 + m, :], ALU.mult)
                kb.tt('dve', t2[:], YSSM[:, m, :], SG[:, m, :], ALU.mult)
                kb.tt('pool', MRG[:, m, :], t1[:], t2[:], ALU.add)
            if b == 0 and tt == dbg_tile and 'mrg' in dbg:
                kb.tap('mrg', MRG[:].rearrange("p a b -> p (a b)"), BF16)
            for m in range(8):
                w = load_wm('wo', m, 8)
                bank = PS[m % 4]
                for kc in range(8):
                    kb.mm(bank[:, :], w[:, kc, :], MRG[:, kc, :], start=(kc == 0), stop=(kc == 7))
                kb.stt(H[:, m, :], bank[:, :], ADA[:, 16 + m, b:b + 1], H[:, m, :], ALU.mult, ALU.add)
        if b == 0 and tt == dbg_tile and 'h1' in dbg:
            kb.tap('h1', H[:].rearrange("p a b -> p (a b)"))
        return H

    def phase6_moe(b, tt, sc, tok0, H):
        with kb.scope() as s6:
            COMBT = kb.sb('combT', [128, T], F32, s6)
            kb.memset('pool', COMBT[:, :], 0.0)
            s6r = kb.scope()
            s6r_st = s6r.__enter__()
            HN2F = kb.sb('hn2f', [128, 8, T], F32, s6r_st)
            with kb.scope() as sn:
                RSTD = rmsnorm_rstd(H, sn)
                TMP = [kb.sb('n2tmp%d' % i, [128, T], F32, sn) for i in range(2)]
                for kc in range(8):
                    tm = TMP[kc % 2]
                    kb.tt('dve', tm[:], H[:, kc, :], RSTD[:], ALU.mult)
                    kb.act(HN2F[:, kc, :], tm[:], AF.Identity, bias=ADA[:, 24 + kc, b:b + 1], scale=GM2[:, kc, b:b + 1])
                    kb.copy('pool', HN[:, kc, :], HN2F[:, kc, :])
            if b == 0 and tt == dbg_tile and 'hn2' in dbg:
                kb.tap('hn2', HN2F[:].rearrange("p a b -> p (a b)"))
            with kb.scope() as sr:
                LG = kb.sb('lg', [128, 36], F32, sr)
                GMX = kb.sb('gmx', [128, 1], F32, sr)
                NGM = kb.sb('ngm', [128, 1], F32, sr)
                GOH = kb.sb('goh', [128, 4], F32, sr)
                EJ = kb.sb('ej', [128, 4], F32, sr)
                SUMG = kb.sb('sumg', [128, 1], F32, sr)
                PG = kb.sb('pg', [128, 1], F32, sr)
                LE8 = kb.sb('le8', [128, 8], F32, sr)
                M8 = kb.sb('m8', [128, 8], F32, sr)
                DD = kb.sb('dd', [128, 1], F32, sr)
                W1_ = kb.sb('w1_', [128, 1], F32, sr)
                A1 = kb.sb('a1', [128, 1], F32, sr)
                A2 = kb.sb('a2', [128, 1], F32, sr)
                C8 = kb.sb('c8', [128, 8], F32, sr)
                C8b = kb.sb('c8b', [128, 8], F32, sr)
                COMB = kb.sb('comb', [128, 32], F32, sr)
                for c in range(4):
                    for kc in range(8):
                        kb.mm(PS[1][:, 0:36], HN2F[:, kc, c * 128:(c + 1) * 128], WR[:, kc, :], start=(kc == 0), stop=(kc == 7))
                    kb.tt('dve', LG[:], PS[1][:, 0:36], BR[:], ALU.add)
                    kb.op('dve', lambda e: e.tensor_reduce(out=GMX[:], in_=LG[:, 0:4], axis=AX.X, op=ALU.max), [LG[:]], [GMX[:]])
                    kb.ts('dve', GOH[:], LG[:, 0:4], GMX[:, 0:1], ALU.is_equal)
                    kb.ts('dve', NGM[:], GMX[:], -1.0, ALU.mult)
                    kb.act(EJ[:], LG[:, 0:4], AF.Exp, bias=NGM[:, 0:1])
                    kb.op('dve', lambda e: e.tensor_reduce(out=SUMG[:], in_=EJ[:], axis=AX.X, op=ALU.add), [EJ[:]], [SUMG[:]])
                    kb.op('dve', lambda e: e.reciprocal(out=PG[:], in_=SUMG[:]), [SUMG[:]], [PG[:]])
                    kb.ts('dve', LE8[:], LG[:, 4:12], GOH[:, 0:1], ALU.mult)
                    for g in range(1, 4):
                        kb.stt(LE8[:], LG[:, 4 + 8 * g:12 + 8 * g], GOH[:, g:g + 1], LE8[:], ALU.mult, ALU.add)
                    kb.op('dve', lambda e: e.max(out=M8[:], in_=LE8[:]), [LE8[:]], [M8[:]])
                    kb.tt('dve', DD[:], M8[:, 1:2], M8[:, 0:1], ALU.subtract)
                    kb.act(DD[:], DD[:], AF.Exp)
                    kb.ts('dve', W1_[:], DD[:], 1.0, ALU.add)
                    kb.op('dve', lambda e: e.reciprocal(out=W1_[:], in_=W1_[:]), [W1_[:]], [W1_[:]])
                    kb.tt('dve', A1[:], W1_[:], PG[:], ALU.mult)
                    kb.tt('dve', A2[:], A1[:], DD[:], ALU.mult)
                    kb.ts('dve', C8[:], LE8[:], M8[:, 0:1], ALU.is_equal, A1[:, 0:1], ALU.mult)
                    kb.ts('dve', C8b[:], LE8[:], M8[:, 1:2], ALU.is_equal, A2[:, 0:1], ALU.mult)
                    kb.tt('dve', C8[:], C8[:], C8b[:], ALU.add)
                    for g in range(4):
                        kb.ts('dve', COMB[:, 8 * g:8 * g + 8], C8[:], GOH[:, g:g + 1], ALU.mult)
                    kb.tr(PS[2][0:32, 0:128], COMB[:], identF[:, :])
                    kb.copy('dve', COMBT[0:32, c * 128:(c + 1) * 128], PS[2][0:32, 0:128])
            s6r.__exit__(None, None, None)
            if b == 0 and tt == dbg_tile and 'comb' in dbg:
                kb.tap('comb', COMBT[0:32, :])
            ACC = kb.sb('acc', [128, 8, T], F32, s6)
            with kb.scope() as se:
                HE = kb.sb('he', [128, 16, T], BF16, se)
                WG = [kb.sb('wg%d' % i, [128, 8, 256], BF16, se) for i in range(2)]
                WU = [kb.sb('wu%d' % i, [128, 8, 256], BF16, se) for i in range(2)]
                WD = [kb.sb('wd%d' % i, [128, 2, 1024], BF16, se) for i in range(2)]
                CBE = [kb.sb('cbe%d' % i, [128, T], F32, se) for i in range(2)]
                SGT = [kb.sb('sgt%d' % i, [128, T], F32, se) for i in range(2)]
                TU = [kb.sb('tu%d' % i, [128, T], F32, se) for i in range(2)]
                for grp in range(4):
                    for e8 in range(8):
                        e = grp * 8 + e8
                        wg, wu = WG[e % 2], WU[e % 2]
                        kb.dma(wg[:].rearrange("p k n -> p (k n)"), scr['wg'][e])
                        kb.dma(wu[:].rearrange("p k n -> p (k n)"), scr['wu'][e])
                        cbe = CBE[e % 2]
                        kb.mm(PS[4][:, :], bc(identF[:, e:e + 1], [128, 128]), COMBT[:, :])
                        kb.copy('act', cbe[:], PS[4][:, :])
                        for j in range(2):
                            bG, bU = PS[2 * j], PS[2 * j + 1]
                            for kc in range(8):
                                kb.mm(bG[:, :], wg[:, kc, j * 128:(j + 1) * 128], HN[:, kc, :], start=(kc == 0), stop=(kc == 7))
                            for kc in range(8):
                                kb.mm(bU[:, :], wu[:, kc, j * 128:(j + 1) * 128], HN[:, kc, :], start=(kc == 0), stop=(kc == 7))
                            sg, tu = SGT[j], TU[j]
                            kb.act(sg[:], bG[:, :], AF.Silu)
                            kb.tt('dve', tu[:], bU[:, :], sg[:], ALU.mult)
                            kb.tt('pool', HE[:, e8 * 2 + j, :], tu[:], cbe[:], ALU.mult)
                    for e8 in range(8):
                        e = grp * 8 + e8
                        wd = WD[e % 2]
                        kb.dma(wd[:].rearrange("p k n -> p (k n)"), scr['wd'][e])
                        for m in range(8):
                            for j in range(2):
                                kb.mm(PS[m][:, :], wd[:, j, m * 128:(m + 1) * 128], HE[:, e8 * 2 + j, :],
                                      start=(e8 == 0 and j == 0), stop=(e8 == 7 and j == 1))
                    for m in range(8):
                        if grp == 0:
                            kb.copy('act', ACC[:, m, :], PS[m][:, :])
                        elif grp < 3:
                            kb.tt('dve', ACC[:, m, :], ACC[:, m, :], PS[m][:, :], ALU.add)
                        else:
                            kb.tt('dve', ACC[:, m, :], ACC[:, m, :], PS[m][:, :], ALU.add)
                            kb.stt(H[:, m, :], ACC[:, m, :], ADA[:, 40 + m, b:b + 1], H[:, m, :], ALU.mult, ALU.add)
            if b == 0 and tt == dbg_tile and 'h2' in dbg:
                kb.tap('h2', H[:].rearrange("p a b -> p (a b)"))
            with kb.scope() as sn:
                RSTD = rmsnorm_rstd(H, sn)
                OUT = ACC
                for kc in range(8):
                    kb.stt(OUT[:, kc, :], H[:, kc, :], VEC[:, 64 + kc:65 + kc], RSTD[:], ALU.mult, ALU.mult)
                ds = kb.dma(outT[:, :, tok0:tok0 + T].rearrange("k p t -> p k t"), OUT[:], dsem='d_out')
                if ds not in kb.out_dsems:
                    kb.out_dsems.append(ds)

    for b in range(nseq):
        kb.mark('seqinit')
        seq_init(b)
        for tt in range(ntiles):
            tok0 = b * SEQ + tt * T
            kb.mark('p1')
            phase1_norm1(b, tt, tok0)
            if stop_phase <= 1:
                continue
            tile_scope = kb.scope()
            st_tile = tile_scope.__enter__()
            COS, SINS = rope_tables(b, tt, st_tile)
            with kb.scope() as sc:
                kb.mark('p2')
                XBC, ZS, DT_T, DTA_T = phase2_ssd_proj(b, tt, sc)
                if stop_phase <= 2:
                    continue
                kb.mark('p3')
                phase3_ssd(b, tt, sc, XBC, ZS, DT_T, DTA_T)
            if stop_phase <= 3:
                continue
            with kb.scope() as sc:
                kb.mark('p4')
                ATTT = phase4_nsa(b, tt, sc, tok0, COS, SINS)
                if stop_phase <= 4:
                    continue
                kb.mark('p5')
                H = phase5_merge(b, tt, sc, tok0, ATTT)
                if stop_phase <= 5:
                    continue
                kb.mark('p6')
                phase6_moe(b, tt, sc, tok0, H)
            tile_scope.__exit__(None, None, None)
    kb.mark('end')
    kb.finish()
    return nc, kb


def core_inputs(inp, shared, b0, nseq):
    x = np.asarray(inp['x'][b0:b0 + nseq], np.float32).reshape(nseq * SEQ, D)
    m = dict(shared)
    m['xT'] = np.ascontiguousarray(x.T).reshape(8, 128, nseq * SEQ)
    c = np.asarray(inp['c'][b0:b0 + nseq], np.float32)
    m['cT'] = np.ascontiguousarray(c.reshape(nseq, 8, 128).transpose(2, 1, 0)).reshape(128, 8 * nseq)
    m['pos'] = np.ascontiguousarray(np.asarray(inp['positions'][b0:b0 + nseq], np.int32))
    return m


_CACHE = {}


def kernel(**inputs):
    B = inputs['x'].shape[0]
    nseq = B // NCORES
    if 'nc' not in _CACHE:
        _CACHE['nc'] = build(nseq)[0]
    nc = _CACHE['nc']
    shared = prep_shared(inputs)
    in_maps = [core_inputs(inputs, shared, i * nseq, nseq) for i in range(NCORES)]
    res = run_bass_kernel_spmd(nc, in_maps, core_ids=list(range(NCORES)))
    out = np.empty((B, SEQ, D), np.float32)
    for i in range(NCORES):
        o = np.asarray(res.results[i]['outT']).reshape(D, nseq * SEQ)
        out[i * nseq:(i + 1) * nseq] = o.T.reshape(nseq, SEQ, D)
    return out
```
